# Optimizing a Trainium2 kernel written in Bass

```python
import math
import jax, jax.numpy as jnp
from jax import lax
import numpy as np

D_MODEL = 1024
BATCH = 2
SEQ = 8192
DEPTH = 1

HEAD_DIM = 64
HEADS_PER_GROUP = 4
DILATED_GROUPS = ((128, 1), (512, 4), (2048, 16))
N_ATTN_HEADS = HEADS_PER_GROUP * len(DILATED_GROUPS)
ATTN_WIDTH = N_ATTN_HEADS * HEAD_DIM
ATTN_OUT_WIDTH = HEADS_PER_GROUP * HEAD_DIM
ATTN_BLOCK = 128
N_REL_BUCKETS = 32
REL_MAX_DISTANCE = 2048
NEG_INF = -1e30

LRU_WIDTH = D_MODEL
LRU_HEADS = 16
LRU_HEAD_DIM = LRU_WIDTH // LRU_HEADS
CONV_WIDTH = 4
LRU_C = 8.0

N_EXPERT_GROUPS = 4
EXPERTS_PER_GROUP = 8
N_EXPERTS = N_EXPERT_GROUPS * EXPERTS_PER_GROUP
TOP_K = 2
D_EXPERT = 512
MOE_BLOCK = 128

EPS = 1e-6
IN_WIDTH = 3 * ATTN_WIDTH + 2 * LRU_WIDTH + 2 * D_MODEL

kernel_name = "hybrid_dilated_attn_rglru_hmoe_block"


def _rmsnorm(x, g):
    xf = x.astype(jnp.float32)
    y = xf * lax.rsqrt(jnp.mean(xf * xf, axis=-1, keepdims=True) + EPS) * g.astype(jnp.float32)
    return y.astype(x.dtype)


def _t5_causal_bucket(dist):
    max_exact = N_REL_BUCKETS // 2
    d_f = jnp.maximum(dist, max_exact).astype(jnp.float32)
    large = max_exact + (jnp.log(d_f / max_exact) / math.log(REL_MAX_DISTANCE / max_exact)
                         * (N_REL_BUCKETS - max_exact)).astype(jnp.int32)
    large = jnp.minimum(large, N_REL_BUCKETS - 1)
    return jnp.where(dist < max_exact, dist, large)


def _dilated_window_attention(q, k, v, rel_bias, window, dilation):
    B, S, H, Dh = q.shape
    nw = window // dilation
    span = dilation * ATTN_BLOCK
    Sp = -(-S // span) * span
    L = Sp // dilation
    nb = L // ATTN_BLOCK

    def to_blocks(t):
        t = jnp.pad(t, ((0, 0), (0, Sp - S), (0, 0), (0, 0)))
        t = t.reshape(B, L, dilation, H, Dh).transpose(0, 2, 3, 1, 4)
        return t.reshape(B, dilation, H, nb, ATTN_BLOCK, Dh)

    def with_prev(t):
        prev = jnp.pad(t[:, :, :, :-1], ((0, 0), (0, 0), (0, 0), (1, 0), (0, 0), (0, 0)))
        return jnp.concatenate([prev, t], axis=4)

    qb = to_blocks(q)
    kb = with_prev(to_blocks(k))
    vb = with_prev(to_blocks(v))

    qi = jnp.arange(ATTN_BLOCK)[:, None]
    ki = jnp.arange(2 * ATTN_BLOCK)[None, :]
    dist = ATTN_BLOCK + qi - ki
    band = (dist >= 0) & (dist <= nw)
    valid = band[None] & ((jnp.arange(nb)[:, None, None] > 0) | (ki[None] >= ATTN_BLOCK))
    bucket = _t5_causal_bucket(jnp.maximum(dist, 0) * dilation)
    bias = rel_bias.astype(jnp.float32)[bucket].transpose(2, 0, 1)

    s = jnp.einsum('brhnqd,brhnkd->brhnqk', qb, kb) * (HEAD_DIM ** -0.5) + bias[None, None, :, None]
    s = jnp.where(valid[None, None, None], s, NEG_INF)
    m = jnp.max(s, axis=-1, keepdims=True)
    p = jnp.exp(s - m)
    l = jnp.sum(p, axis=-1)
    o = jnp.einsum('brhnqk,brhnkd->brhnqd', p, vb) / l[..., None]
    lse = m[..., 0] + jnp.log(l)

    o = o.reshape(B, dilation, H, L, Dh).transpose(0, 3, 1, 2, 4).reshape(B, Sp, H, Dh)[:, :S]
    lse = lse.reshape(B, dilation, H, L).transpose(0, 3, 1, 2).reshape(B, Sp, H)[:, :S]
    return o, lse


def _dilated_attention_mixer(qkv, rel_bias):
    B, S, _ = qkv.shape
    qkv = qkv.astype(jnp.float32).reshape(B, S, 3, N_ATTN_HEADS, HEAD_DIM)
    q, k, v = qkv[:, :, 0], qkv[:, :, 1], qkv[:, :, 2]
    outs, lses = [], []
    for gi, (window, dilation) in enumerate(DILATED_GROUPS):
        hs = slice(gi * HEADS_PER_GROUP, (gi + 1) * HEADS_PER_GROUP)
        o, lse = _dilated_window_attention(q[:, :, hs], k[:, :, hs], v[:, :, hs], rel_bias[:, hs], window, dilation)
        outs.append(o)
        lses.append(lse)
    o = jnp.stack(outs)
    wts = jax.nn.softmax(jnp.stack(lses), axis=0)
    return jnp.sum(wts[..., None] * o, axis=0).reshape(B, S, ATTN_OUT_WIDTH)


def _rg_lru_mixer(xr, gate_in, conv_w, conv_b, w_rg, b_rg, w_ig, b_ig, lam):
    B, S, W = xr.shape
    xp = jnp.pad(xr, ((0, 0), (CONV_WIDTH - 1, 0), (0, 0)))
    xc = sum(xp[:, j:j + S] * conv_w[j] for j in range(CONV_WIDTH)) + conv_b
    xh = xc.reshape(B, S, LRU_HEADS, LRU_HEAD_DIM)
    r = jax.nn.sigmoid(jnp.einsum('bshi,hij->bshj', xh, w_rg).reshape(B, S, W) + b_rg)
    i = jax.nn.sigmoid(jnp.einsum('bshi,hij->bshj', xh, w_ig).reshape(B, S, W) + b_ig)
    log_a = -LRU_C * r.astype(jnp.float32) * jax.nn.softplus(-lam.astype(jnp.float32))
    a = jnp.exp(log_a)
    b = jnp.sqrt(-jnp.expm1(2.0 * log_a)) * (i * xc).astype(jnp.float32)

    def combine(c1, c2):
        a1, b1 = c1
        a2, b2 = c2
        return a1 * a2, a2 * b1 + b2

    _, h = lax.associative_scan(combine, (a, b), axis=1)
    return h.astype(xr.dtype) * jax.nn.gelu(gate_in)


def _hierarchical_moe(h, w_router_group, w_router_expert, w_gate_up, w_down):
    B, S, D = h.shape
    T = B * S
    ht = h.reshape(T, D)
    hf = ht.astype(jnp.float32)
    p_group = jax.nn.softmax(hf @ w_router_group.astype(jnp.float32), axis=-1)
    g_sel = jnp.argmax(p_group, axis=-1)
    p_sel = jnp.take_along_axis(p_group, g_sel[:, None], axis=1)[:, 0]
    logits_e = (hf @ w_router_expert.astype(jnp.float32)).reshape(T, N_EXPERT_GROUPS, EXPERTS_PER_GROUP)
    logits_sel = jnp.take_along_axis(logits_e, g_sel[:, None, None], axis=1)[:, 0]
    top_vals, top_idx = lax.top_k(logits_sel, TOP_K)
    gate = jax.nn.softmax(top_vals, axis=-1) * p_sel[:, None]
    expert = g_sel[:, None] * EXPERTS_PER_GROUP + top_idx

    TK = T * TOP_K
    e_flat = expert.reshape(TK).astype(jnp.int32)
    tok_flat = jnp.repeat(jnp.arange(T, dtype=jnp.int32), TOP_K)
    w_flat = gate.reshape(TK)
    order = jnp.argsort(e_flat)
    e_s, tok_s, w_s = e_flat[order], tok_flat[order], w_flat[order]
    counts = jnp.bincount(e_flat, length=N_EXPERTS)
    start = jnp.cumsum(counts) - counts
    padded = ((counts + MOE_BLOCK - 1) // MOE_BLOCK) * MOE_BLOCK
    pend = jnp.cumsum(padded)
    pstart = pend - padded
    dest = pstart[e_s] + (jnp.arange(TK, dtype=jnp.int32) - start[e_s])
    P = TK + N_EXPERTS * MOE_BLOCK
    nblk = P // MOE_BLOCK
    buf_tok = jnp.full((P,), T, dtype=jnp.int32).at[dest].set(tok_s)
    buf_w = jnp.zeros((P,), jnp.float32).at[dest].set(w_s)
    block_expert = jnp.minimum(jnp.searchsorted(pend, jnp.arange(nblk) * MOE_BLOCK, side='right'), N_EXPERTS - 1)
    ht_pad = jnp.concatenate([ht, jnp.zeros((1, D), ht.dtype)], axis=0)

    def block_fn(args):
        idx, e = args
        xb = ht_pad[idx]
        gu = xb @ w_gate_up[e]
        g, u = jnp.split(gu, 2, axis=-1)
        return (jax.nn.silu(g) * u) @ w_down[e]

    y_buf = lax.map(block_fn, (buf_tok.reshape(nblk, MOE_BLOCK), block_expert)).reshape(P, D)
    y = jax.ops.segment_sum(y_buf * buf_w[:, None].astype(y_buf.dtype), buf_tok, num_segments=T + 1)[:T]
    return y.reshape(B, S, D).astype(h.dtype)


def setup_inputs(seed: int = 0) -> dict:
    key = jax.random.key(seed)
    ks = jax.random.split(key, 24)
    f32 = jnp.float32
    nrm = lambda k, shape, scale: jax.random.normal(k, shape, f32) * scale
    a0 = jax.random.uniform(ks[10], (DEPTH, LRU_WIDTH), f32, minval=0.9, maxval=0.999)
    s0 = a0 ** (1.0 / LRU_C)
    return {
        "x": nrm(ks[0], (BATCH, SEQ, D_MODEL), 1.0),
        "rel_bias": nrm(ks[1], (N_REL_BUCKETS, N_ATTN_HEADS), 0.5),
        "norm1": 1.0 + nrm(ks[2], (DEPTH, D_MODEL), 0.02),
        "w_in": nrm(ks[3], (DEPTH, D_MODEL, IN_WIDTH), D_MODEL ** -0.5),
        "conv_w": nrm(ks[4], (DEPTH, CONV_WIDTH, LRU_WIDTH), CONV_WIDTH ** -0.5),
        "conv_b": nrm(ks[5], (DEPTH, LRU_WIDTH), 0.02),
        "w_rg": nrm(ks[6], (DEPTH, LRU_HEADS, LRU_HEAD_DIM, LRU_HEAD_DIM), LRU_HEAD_DIM ** -0.5),
        "b_rg": nrm(ks[7], (DEPTH, LRU_WIDTH), 0.1),
        "w_ig": nrm(ks[8], (DEPTH, LRU_HEADS, LRU_HEAD_DIM, LRU_HEAD_DIM), LRU_HEAD_DIM ** -0.5),
        "b_ig": nrm(ks[9], (DEPTH, LRU_WIDTH), 0.1),
        "lru_lambda": jnp.log(s0) - jnp.log1p(-s0),
        "w_proj_attn": nrm(ks[11], (DEPTH, ATTN_OUT_WIDTH, D_MODEL), ATTN_OUT_WIDTH ** -0.5),
        "w_proj_lru": nrm(ks[12], (DEPTH, LRU_WIDTH, D_MODEL), LRU_WIDTH ** -0.5),
        "w_out": nrm(ks[13], (DEPTH, D_MODEL, D_MODEL), D_MODEL ** -0.5),
        "norm2": 1.0 + nrm(ks[14], (DEPTH, D_MODEL), 0.02),
        "w_router_group": nrm(ks[15], (DEPTH, D_MODEL, N_EXPERT_GROUPS), D_MODEL ** -0.5),
        "w_router_expert": nrm(ks[16], (DEPTH, D_MODEL, N_EXPERTS), D_MODEL ** -0.5),
        "w_gate_up": nrm(ks[17], (DEPTH, N_EXPERTS, D_MODEL, 2 * D_EXPERT), D_MODEL ** -0.5),
        "w_down": nrm(ks[18], (DEPTH, N_EXPERTS, D_EXPERT, D_MODEL), D_EXPERT ** -0.5),
        "norm_f": 1.0 + nrm(ks[19], (D_MODEL,), 0.02),
    }


def reference(x, rel_bias, norm1, w_in, conv_w, conv_b, w_rg, b_rg, w_ig, b_ig, lru_lambda,
              w_proj_attn, w_proj_lru, w_out, norm2, w_router_group, w_router_expert,
              w_gate_up, w_down, norm_f):
    for layer in range(DEPTH):
        h = _rmsnorm(x, norm1[layer])
        proj = h @ w_in[layer]
        c1 = 3 * ATTN_WIDTH
        c2 = c1 + LRU_WIDTH
        c3 = c2 + LRU_WIDTH
        qkv, xr, g_lru, g_merge = proj[..., :c1], proj[..., c1:c2], proj[..., c2:c3], proj[..., c3:]
        attn = _dilated_attention_mixer(qkv, rel_bias).astype(x.dtype)
        lru = _rg_lru_mixer(xr, g_lru, conv_w[layer], conv_b[layer], w_rg[layer], b_rg[layer],
                            w_ig[layer], b_ig[layer], lru_lambda[layer])
        gate_a, gate_b = jnp.split(jax.nn.sigmoid(g_merge), 2, axis=-1)
        merged = gate_a * (attn @ w_proj_attn[layer]) + gate_b * (lru @ w_proj_lru[layer])
        x = x + merged @ w_out[layer]
        h2 = _rmsnorm(x, norm2[layer])
        x = x + _hierarchical_moe(h2, w_router_group[layer], w_router_expert[layer],
                                  w_gate_up[layer], w_down[layer])
    return _rmsnorm(x, norm_f)
```

```python
import math
from contextlib import ExitStack

import numpy as np
import concourse.bass as bass
import concourse.mybir as mybir
from concourse.bass_utils import run_bass_kernel_spmd

F32 = mybir.dt.float32
BF16 = mybir.dt.bfloat16
U32 = mybir.dt.uint32
AF = mybir.ActivationFunctionType
ALU = mybir.AluOpType
AX = mybir.AxisListType

D = 1024
TOK = 2048
NCH = 4
C1 = 2304
C2 = 3328
C3 = 4352
DIL = (1, 4, 16)
NEG = -30000.0
EPS = 1e-6
CAP = 256
NEXP = 32


class Buf:
    __slots__ = ("name", "w", "r")

    def __init__(self, name):
        self.name = name
        self.w = None
        self.r = []


class V:
    __slots__ = ("ap", "buf")

    def __init__(self, ap, buf):
        self.ap = ap
        self.buf = buf

    def __getitem__(self, i):
        return V(self.ap[i], self.buf)

    def rr(self, pat, **kw):
        return V(self.ap.rearrange(pat, **kw), self.buf)

    def bc(self, dt):
        return V(self.ap.bitcast(dt), self.buf)

    def bt(self, shape):
        return V(self.ap.broadcast_to(shape), self.buf)

    def un(self, ax):
        return V(self.ap.unsqueeze(ax), self.buf)

    def tag(self, buf):
        return V(self.ap, buf)


def sstep(start, n, step):
    return slice(start, start + (n - 1) * step + 1, step)


def _ap(x):
    return x.ap if isinstance(x, V) else x


class Prog:
    ENGS = ("pe", "dve", "act", "pool", "sp")

    def __init__(self, nc, stack):
        self.nc = nc
        self.stack = stack
        self.sem = {e: stack.enter_context(nc.semaphore("sem_" + e)) for e in self.ENGS}
        self.cnt = {e: 0 for e in self.ENGS}
        self.stream = {e: [] for e in self.ENGS}
        self.waited = {e: {} for e in self.ENGS}
        self.dsems = {}
        self.semobj = {("e", e): self.sem[e] for e in self.ENGS}
        self.bufs = []
        self.same_engine_sync = True

    def buf(self, name):
        b = Buf(name)
        self.bufs.append(b)
        return b

    def dsem(self, name):
        if name not in self.dsems:
            h = self.stack.enter_context(self.nc.semaphore("d_" + name))
            self.dsems[name] = [h, 0]
            self.semobj[("d", name)] = h
        return self.dsems[name]

    def _collect(self, eng, ins, outs):
        ev = []
        for v in ins:
            if isinstance(v, V) and v.buf is not None and v.buf.w is not None:
                ev.append(v.buf.w)
        for v in outs:
            b = v.buf
            if b is None:
                continue
            if b.w is not None:
                ev.append(b.w)
            ev.extend(b.r)
        waits = []
        wd = self.waited[eng]
        mx = {}
        for key, val in ev:
            if key == ("e", eng) and (eng == "pe" or not self.same_engine_sync):
                continue
            if mx.get(key, 0) < val:
                mx[key] = val
        for key, val in mx.items():
            if wd.get(key, 0) >= val:
                continue
            wd[key] = val
            waits.append((key, val))
        return waits

    def _update(self, token, ins, outs):
        for v in outs:
            if v.buf is not None:
                v.buf.w = token
                v.buf.r = []
        for v in ins:
            if isinstance(v, V) and v.buf is not None:
                v.buf.r.append(token)
                if len(v.buf.r) > 64:
                    v.buf.r = v.buf.r[-64:] if False else v.buf.r

    def op(self, eng, fn, outs, ins):
        waits = self._collect(eng, ins, outs)
        self.cnt[eng] += 1
        token = (("e", eng), self.cnt[eng])
        self.stream[eng].append((waits, fn, self.sem[eng], 1))
        self._update(token, ins, outs)

    def dma(self, queue, out, in_, dname, fn=None, extra=()):
        waits = self._collect(queue, [in_] + list(extra), [out])
        d = self.dsem(dname)
        d[1] += 16
        token = (("d", dname), d[1])
        if fn is None:
            o, i = out.ap, in_.ap
            fn = lambda e: e.dma_start(out=o, in_=i)
        self.stream[queue].append((waits, fn, d[0], 16))
        self._update(token, [in_] + list(extra), [out])

    def barrier(self):
        targets = [(("e", e), self.cnt[e]) for e in self.ENGS if self.cnt[e] > 0]
        targets += [(("d", n), d[1]) for n, d in self.dsems.items() if d[1] > 0]
        for eng in self.ENGS:
            waits = []
            wd = self.waited[eng]
            for key, val in targets:
                if key == ("e", eng):
                    continue
                if wd.get(key, 0) >= val:
                    continue
                wd[key] = val
                waits.append((key, val))
            if waits:
                self.stream[eng].append((waits, None, None, 0))
        for b in self.bufs:
            b.w = None
            b.r = []

    def replay(self, eng, e):
        for waits, fn, sem, inc in self.stream[eng]:
            for key, val in waits:
                e.wait_ge(self.semobj[key], val)
            if fn is not None:
                ins = fn(e)
                ins.then_inc(sem, inc)

    def mm(self, out, lhsT, rhs, start=True, stop=True):
        o, l, r = out.ap, lhsT.ap, rhs.ap
        self.op("pe", lambda e: e.matmul(o, l, r, start=start, stop=stop), [out], [lhsT, rhs])

    def tr(self, out, in_, ident):
        o, i, d = out.ap, in_.ap, ident.ap
        self.op("pe", lambda e: e.transpose(o, i, d), [out], [in_, ident])

    def act(self, out, in_, func, bias=None, scale=None, accum=None):
        kw = {}
        ins = [in_]
        outs = [out]
        if bias is not None:
            kw["bias"] = _ap(bias)
            ins.append(bias)
        if scale is not None:
            kw["scale"] = _ap(scale)
            ins.append(scale)
        if accum is not None:
            kw["accum_out"] = accum.ap
            outs.append(accum)
        o, i = out.ap, in_.ap
        self.op("act", lambda e: e.activation(o, i, func, **kw), outs, ins)

    def ts(self, eng, out, in0, s1, s2, op0, op1=None):
        o, i = out.ap, in0.ap
        a1, a2 = _ap(s1), _ap(s2)
        if op1 is None:
            fn = lambda e: e.tensor_scalar(o, i, a1, None, op0)
        else:
            fn = lambda e: e.tensor_scalar(o, i, a1, a2, op0, op1)
        self.op(eng, fn, [out], [in0, s1, s2])

    def stt(self, eng, out, in0, scalar, in1, op0, op1):
        o, i0, i1, s = out.ap, in0.ap, in1.ap, _ap(scalar)
        self.op(eng, lambda e: e.scalar_tensor_tensor(o, i0, s, i1, op0, op1), [out], [in0, scalar, in1])

    def tt(self, eng, out, in0, in1, op):
        o, i0, i1 = out.ap, in0.ap, in1.ap
        self.op(eng, lambda e: e.tensor_tensor(o, i0, i1, op), [out], [in0, in1])

    def cp(self, eng, out, in_):
        o, i = out.ap, in_.ap
        if eng == "act":
            self.op(eng, lambda e: e.copy(o, i), [out], [in_])
        else:
            self.op(eng, lambda e: e.tensor_copy(o, i), [out], [in_])

    def red(self, out, in_, op, negate=False):
        o, i = out.ap, in_.ap
        self.op("dve", lambda e: e.tensor_reduce(o, i, AX.X, op, negate=negate), [out], [in_])

    def recip(self, out, in_):
        o, i = out.ap, in_.ap
        self.op("dve", lambda e: e.reciprocal(o, i), [out], [in_])

    def scan(self, out, d0, d1, init, op0, op1):
        o, a, b, c = out.ap, d0.ap, d1.ap, _ap(init)
        self.op("dve", lambda e: e.tensor_tensor_scan(o, a, b, c, op0, op1), [out], [d0, d1, init])

    def memset(self, eng, out, val):
        o = out.ap
        self.op(eng, lambda e: e.memset(o, val), [out], [])


class _Stop(Exception):
    pass


MARKS = []


def build_program(stage=99, dump=False):
    nc = bass.Bass("TRN2", target_bir_lowering=False)

    def din(name, shape, dt=F32):
        return nc.dram_tensor(name, shape, dt, kind="ExternalInput").ap()

    xin_d = din("xin", [NCH, TOK, D])
    tabs_d = din("tabs", [3, 128, 2048])
    valid_d = din("valid", [128, 4])
    cvec_d = din("cvec", [128, 64])
    n1_d = din("norm1", [1, D])
    n2_d = din("norm2", [1, D])
    nf_d = din("normf", [1, D])
    w_in_d = din("w_in", [D, 6400])
    wbd_d = din("wbd", [128, 2 * 8 * 128])
    wpa_d = din("w_pa", [256, D])
    wpl_d = din("w_pl", [D, D])
    wout_d = din("w_out", [D, D])
    wr_d = din("wr", [128, 8 * 36])
    wgu_d = din("w_gu", [NEXP, D, D])
    wdn_d = din("w_dn", [NEXP, 512, D])
    kcb_d = din("kc_bf", [128, 640])
    kcf_d = din("kc_f", [128, 160])
    out_d = nc.dram_tensor("out", [TOK, D], F32, kind="ExternalOutput").ap()
    xbuf_d = nc.dram_tensor("xbuf", [NEXP * CAP, D], BF16, kind="Internal").ap()
    ybuf_d = nc.dram_tensor("ybuf", [NEXP * CAP, D], F32, kind="Internal").ap()
    if dump:
        d_r1 = nc.dram_tensor("d_r1", [128, 16384], F32, kind="ExternalOutput").ap()
        d_r2 = nc.dram_tensor("d_r2", [128, 16384], F32, kind="ExternalOutput").ap()
        d_r3 = nc.dram_tensor("d_r3", [128, 6144], F32, kind="ExternalOutput").ap()
        d_sm = nc.dram_tensor("d_sm", [128, 512], F32, kind="ExternalOutput").ap()
        d_idx = nc.dram_tensor("d_idx", [128, 32], U32, kind="ExternalOutput").ap()

    with ExitStack() as stack:
        P = Prog(nc, stack)

        def sb(name, shape, dt):
            t = stack.enter_context(nc.sbuf_tensor(name, shape, dt))
            return t

        def ps(name, shape, dt):
            return stack.enter_context(nc.psum_tensor(name, shape, dt))

        def dram(ap, name):
            return V(ap, P.buf(name))

        xin = dram(xin_d, "xin")
        tabs = dram(tabs_d, "tabs")
        w_in = dram(w_in_d, "w_in")
        wgu = dram(wgu_d, "wgu")
        wdn = dram(wdn_d, "wdn")
        outv = dram(out_d, "out")
        xbuf = dram(xbuf_d, "xbuf")
        ybuf = dram(ybuf_d, "ybuf")

        R1 = sb("R1", [128, 16384], F32)
        R2 = sb("R2", [128, 16384], F32)
        R3 = sb("R3", [128, 6144], F32)
        WS = [sb(f"ws{i}", [128, 8, 512], BF16) for i in range(2)]
        XS = [sb(f"xs{i}", [128, 1024], F32) for i in range(4)]
        XNB = [sb(f"xnb{i}", [128, 1024], BF16) for i in range(4)]
        GBC = sb("gbct", [128, 1024], F32)
        CVt = sb("cv", [128, 64], F32)
        VALt = sb("val", [128, 4], F32)
        KBt = sb("kb", [128, 640], BF16)
        KFt = sb("kf", [128, 160], F32)
        WBDt = sb("wbdt", [128, 2 * 8 * 128], BF16)
        WRt = sb("wrt", [128, 8 * 36], F32)
        SMt = sb("sm", [128, 512], F32)
        IDXt = sb("idx", [128, 32], U32)

        PSt = [ps(f"ps{i}", [128, 512], F32) for i in range(7)]
        PTt = ps("pst", [128, 1024], BF16)
        PS = [V(PSt[i][:, :], P.buf(f"ps{i}")) for i in range(7)]
        PT_bufs = [P.buf(f"pst{i}") for i in range(4)]
        PT = [V(PTt[:, i * 256:(i + 1) * 256], PT_bufs[i]) for i in range(4)]
        PTall = V(PTt[:, :], PT_bufs[0])

        def region(t, c0, ncol, name, dt=F32, shape=None):
            ap = t[:, c0:c0 + ncol]
            if dt != F32:
                ap = ap.bitcast(dt)
            v = V(ap, P.buf(name))
            return v

        CV = V(CVt[:, :], P.buf("cv"))
        VAL = V(VALt[:, :], P.buf("val"))
        KB = V(KBt[:, :], P.buf("kb"))
        KF = V(KFt[:, :], P.buf("kf"))
        WBD = V(WBDt[:, :], P.buf("wbd"))
        WR = V(WRt[:, :], P.buf("wr"))
        gbc = V(GBC[:, :], P.buf("gbc"))
        ws = [V(WS[i][:, :, :], P.buf(f"ws{i}")) for i in range(2)]
        xs = [V(XS[i][:, :], P.buf(f"xs{i}")) for i in range(4)]
        xnb = [V(XNB[i][:, :], P.buf(f"xnb{i}")) for i in range(4)]
        IDX = V(IDXt[:, :], P.buf("idx"))

        IDENT_B = KB[:, 0:128]
        UMAT = KB[:, 128:256]
        ONES_B = KB[:, 256:384]
        EBC = KB[:, 384:640]
        IDENT_F = KF[:, 0:128]
        EOFF = KF[:, 128:160]

        sm_off = [0]

        def small(n, name):
            v = V(SMt[:, sm_off[0]:sm_off[0] + n], P.buf(name))
            sm_off[0] += n
            return v

        ST = small(8, "st")
        XTAIL = small(32, "xtail")
        NSP = small(16, "nsp")
        HB = small(16, "hb")
        HVAL = small(4, "hval")
        G12 = small(32, "g12")
        CNT = small(32, "cnt")
        stat = [small(8, f"stat{i}") for i in range(4)]
        ATT = [small(16, f"att{i}") for i in range(2)]
        RT = [small(128, f"rt{i}") for i in range(2)]

        P.dma("sp", CV, dram(cvec_d, "cvec_d"), "c_cv")
        P.dma("sp", VAL, dram(valid_d, "valid_d"), "c_val")
        P.dma("sp", KF, dram(kcf_d, "kcf_d"), "c_kf")
        P.dma("sp", WR, dram(wr_d, "wr_d"), "c_wr")
        P.dma("pool", KB, dram(kcb_d, "kcb_d"), "c_kb")
        P.dma("pool", WBD, dram(wbd_d, "wbd_d"), "c_wbd")
        P.memset("dve", ST, 0.0)
        P.memset("dve", XTAIL, 0.0)
        P.memset("dve", CNT, 0.0)
        P.act(NSP[:, 0:8], CV[:, 56:64], AF.Exp, scale=-1.0)
        P.act(NSP[:, 0:8], NSP[:, 0:8], AF.Ln, bias=1.0)
        P.ts("dve", NSP[:, 8:16], NSP[:, 0:8], -8.0, None, ALU.mult)
        P.ts("dve", NSP[:, 0:8], NSP[:, 0:8], -4.0, None, ALU.mult)
        P.ts("dve", HB[:, 0:8], CV[:, 40:48], 0.5, None, ALU.mult)
        P.ts("dve", HB[:, 8:16], CV[:, 48:56], 0.5, None, ALU.mult)
        P.ts("dve", HVAL, VAL, 0.5, None, ALU.mult)

        CW = CV[:, 0:32].rr("p (j c) -> p j c", j=4)
        CB = CV[:, 32:40]
        BRG = CV[:, 40:48]
        BIG_ = CV[:, 48:56]
        WBDv = WBD.rr("p (g c o) -> p g c o", g=2, c=8)

        hT = V(R1[:, 0:8192].bitcast(BF16).rearrange("p (k t) -> p k t", k=8), P.buf("hT"))
        lruT = V(R1[:, 8192:16384].bitcast(BF16).rearrange("p (k t) -> p k t", k=8), P.buf("lruT"))
        x1buf = P.buf("x1")
        x1 = V(R1[:, :].rearrange("p (t c) -> p t c", t=16), x1buf)

        r3b = R3[:, :].bitcast(BF16)
        kTh = []
        Vh = []
        off = 0
        for g in range(3):
            ext = 128 * DIL[g]
            kTh.append(V(r3b[:, off:off + 2 * ext].rearrange("p (c t) -> p c t", c=2), P.buf(f"kTh{g}")))
            off += 2 * ext
        for g in range(3):
            d = DIL[g]
            Vh.append(V(r3b[:, off:off + d * 256].rearrange("p (r c) -> p r c", r=d), P.buf(f"Vh{g}")))
            off += d * 256

        sctr = [0]

        def next_stat():
            sctr[0] += 1
            return stat[sctr[0] % 4]

        def load_gbc(src_d, nm):
            P.dma("sp", gbc, V(src_d.broadcast_to([128, D]), P.buf(nm)), "gbc")

        def rms_tile(src, dst, gb, slot_scr):
            s = next_stat()
            P.act(slot_scr, src, AF.Square, accum=s[:, 0:1])
            P.act(s[:, 1:2], s[:, 0:1], AF.Sqrt, bias=EPS, scale=1.0 / D)
            P.recip(s[:, 2:3], s[:, 1:2])
            P.stt("dve", dst, src, s[:, 2:3], gb, ALU.mult, ALU.mult)

        wsl = [0]

        def load_w(col_list, width=None):
            s = wsl[0] % 2
            wsl[0] += 1
            for (c0, n, d0) in col_list:
                src = w_in[:, c0:c0 + n].rr("(k p) c -> p k c", p=128)
                P.dma("pool", ws[s][:, :, d0:d0 + n], src, f"ws{s}")
            return ws[s]

        psr = [0]

        def next_ps(lo=0, hi=7):
            psr[0] += 1
            return PS[lo + psr[0] % (hi - lo)]

        def lru_views():
            sets = []
            o = 0
            for s in range(2):
                d_ = {}
                d_["XR"] = region(R2, o, 1032, f"XR{s}"); o += 1032
                d_["XC"] = region(R2, o, 1024, f"XC{s}"); o += 1024
                d_["RA"] = region(R2, o, 1024, f"RA{s}"); o += 1024
                d_["II"] = region(R2, o, 1024, f"II{s}"); o += 1024
                d_["TT"] = region(R2, o, 1024, f"TT{s}"); o += 1024
                d_["GL"] = region(R2, o, 1024, f"GL{s}"); o += 1024
                d_["XCB"] = region(R2, o, 512, f"XCB{s}", BF16); o += 512
                sets.append(d_)
            return sets

        def norm_chunk(q):
            def front(tt):
                sl = tt % 4
                P.dma("sp", xs[sl], xin[q, tt * 128:(tt + 1) * 128, :], f"xs{sl}")
                rms_tile(xs[sl], xnb[sl], gbc, xnb[sl])

            def back(tt):
                sl = tt % 4
                for k in range(8):
                    P.tr(PTall[:, k * 128:(k + 1) * 128], xnb[sl][:, k * 128:(k + 1) * 128], IDENT_B)
                if tt % 2:
                    P.cp("act", hT[:, :, tt * 128:(tt + 1) * 128], PTall.rr("p (k t) -> p k t", k=8))
                else:
                    P.cp("dve", hT[:, :, tt * 128:(tt + 1) * 128], PTall.rr("p (k t) -> p k t", k=8))

            front(0)
            front(1)
            for tt in range(16):
                if tt + 2 < 16:
                    front(tt + 2)
                back(tt)

        def lru_chunk(q, sets, own):
            state = {"wcur": None, "wgl": None}

            def S1(u):
                c, hf = u // 2, u % 2
                cc = c % 4
                XR = sets[u % 2]["XR"]
                t0 = hf * 1024
                if c % 4 == 0 and hf == 0:
                    state["wcur"] = load_w([(C1 + (c // 4) * 512, 512, 0)])
                wcur = state["wcur"]
                for j in range(2):
                    pp = next_ps(0, 3)
                    for k in range(8):
                        P.mm(pp, wcur[:, k, cc * 128:(cc + 1) * 128], hT[:, k, t0 + j * 512:t0 + (j + 1) * 512],
                             start=(k == 0), stop=(k == 7))
                    P.cp("act", XR[:, 3 + j * 512:3 + (j + 1) * 512], pp)

            def S2(u):
                c = u // 2
                S = sets[u % 2]
                XR, XC = S["XR"], S["XC"]
                P.cp("dve", XR[:, 0:3], XTAIL[:, c * 4:c * 4 + 3])
                P.ts("dve", XC, XR[:, 3:1027], CW[:, 3, c:c + 1], CB[:, c:c + 1], ALU.mult, ALU.add)
                for j in range(3):
                    P.stt("dve", XC, XR[:, j:j + 1024], CW[:, j, c:c + 1], XC, ALU.mult, ALU.add)
                P.cp("dve", XTAIL[:, c * 4:c * 4 + 3], XR[:, 1024:1027])

            def S3a(u):
                S = sets[u % 2]
                P.cp("pool", S["XCB"], S["XC"])

            def S3b(u):
                c = u // 2
                XCB = sets[u % 2]["XCB"]
                for j in range(2):
                    P.mm(PS[3 + j], WBDv[:, 0, c, :], XCB[:, j * 512:(j + 1) * 512])
                    P.mm(PS[5 + j], WBDv[:, 1, c, :], XCB[:, j * 512:(j + 1) * 512])

            def S4(u):
                c = u // 2
                S = sets[u % 2]
                RA, II, TT = S["RA"], S["II"], S["TT"]
                for j in range(2):
                    P.act(RA[:, j * 512:(j + 1) * 512], PS[3 + j], AF.Tanh, bias=HB[:, c:c + 1], scale=0.5)
                    P.act(II[:, j * 512:(j + 1) * 512], PS[5 + j], AF.Tanh, bias=HB[:, 8 + c:9 + c], scale=0.5)
                P.act(TT, RA, AF.Exp, scale=NSP[:, 8 + c:9 + c], bias=NSP[:, 8 + c:9 + c])
                P.act(RA, RA, AF.Exp, scale=NSP[:, c:c + 1], bias=NSP[:, c:c + 1])
                P.act(TT, TT, AF.Sqrt, bias=1.0, scale=-1.0)

            def S5(u):
                c, hf = u // 2, u % 2
                cc = c % 4
                S = sets[u % 2]
                XC, RA, II, TT, GL = S["XC"], S["RA"], S["II"], S["TT"], S["GL"]
                t0 = hf * 1024
                P.stt("dve", II, II, 1.0, XC, ALU.add, ALU.mult)
                P.stt("dve", II, II, HVAL[:, q:q + 1], TT, ALU.mult, ALU.mult)
                P.scan(XC, RA, II, ST[:, c:c + 1], ALU.mult, ALU.add)
                P.cp("dve", ST[:, c:c + 1], XC[:, 1023:1024])
                if own:
                    if c % 4 == 0 and hf == 0:
                        state["wgl"] = load_w([(C2 + (c // 4) * 512, 512, 0)])
                    wgl = state["wgl"]
                    for j in range(2):
                        pp = next_ps(0, 3)
                        for k in range(8):
                            P.mm(pp, wgl[:, k, cc * 128:(cc + 1) * 128], hT[:, k, t0 + j * 512:t0 + (j + 1) * 512],
                                 start=(k == 0), stop=(k == 7))
                        P.act(GL[:, j * 512:(j + 1) * 512], pp, AF.Gelu)
                    P.tt("dve", lruT[:, c, t0:t0 + 1024], XC, GL, ALU.mult)

            S1(0)
            S2(0)
            S3a(0)
            for u in range(16):
                if u + 1 < 16:
                    S1(u + 1)
                S3b(u)
                if u + 1 < 16:
                    S2(u + 1)
                S4(u)
                if u + 1 < 16:
                    S3a(u + 1)
                S5(u)

        def halo_kv():
            for g in range(3):
                d = DIL[g]
                ext = 128 * d
                h0 = TOK - ext
                w = load_w([(768 + g * 256, 256, 0), (1536 + g * 256, 256, 256)])
                for cb in range(2):
                    for t0 in range(0, ext, 512):
                        n = min(512, ext - t0)
                        pp = next_ps(0, 3)
                        for k in range(8):
                            P.mm(pp[:, 0:n], w[:, k, cb * 128:(cb + 1) * 128], hT[:, k, h0 + t0:h0 + t0 + n],
                                 start=(k == 0), stop=(k == 7))
                        P.cp("act", kTh[g][:, cb, t0:t0 + n], pp[:, 0:n])
                for r in range(d):
                    pp = next_ps(0, 3)
                    for k in range(8):
                        P.mm(pp[:, 0:256], hT[:, k, sstep(h0 + r, 128, d)], w[:, k, 256:512],
                             start=(k == 0), stop=(k == 7))
                    P.cp("dve", Vh[g][:, r, :], pp[:, 0:256])

        def check(n):
            MARKS.append((n, dict(P.cnt)))
            if stage == n:
                raise _Stop()

        try:
            load_gbc(n1_d, "n1d")
            sets = lru_views()
            for q in range(3):
                norm_chunk(q)
                if q == 0:
                    check(1)
                lru_chunk(q, sets, own=False)
                if q == 0:
                    check(2)
                if q == 2:
                    halo_kv()
            norm_chunk(3)
            P.barrier()
            check(3)

            qT = V(R2[:, 0:2048].bitcast(BF16).rearrange("p (c t) -> p c t", c=2), P.buf("qT"))
            kT = V(R2[:, 2048:4096].bitcast(BF16).rearrange("p (c t) -> p c t", c=2), P.buf("kT"))
            Vo = V(R2[:, 4096:6144].bitcast(BF16).rearrange("p (r c) -> p r c", r=16), P.buf("Vo"))
            TAB = V(R2[:, 6144:8192].rearrange("p (v h k) -> p v h k", v=2, h=4), P.buf("TAB"))
            LT = [V(R2[:, 8192 + g * 2048:8192 + (g + 1) * 2048], P.buf(f"LT{g}")) for g in range(3)]
            o2 = 14336
            S2 = [region(R2, o2 + i * 256, 256, f"S2_{i}") for i in range(2)]
            o2 += 512
            Pb = [region(R2, o2 + i * 128, 128, f"P_{i}", BF16) for i in range(2)]
            o2 += 256
            PTs = [region(R2, o2 + i * 128, 128, f"PTs_{i}", BF16) for i in range(2)]
            o2 += 256
            On = [region(R2, o2 + i * 384, 384, f"On_{i}") for i in range(2)]
            o2 += 768
            for i in range(2):
                P.memset("dve", On[i], 0.0)
            OT = [V(lruT.ap[:, 2 * g:2 * g + 2, :], lruT.buf) for g in range(3)]

            for g in range(3):
                d = DIL[g]
                nblk = 16 // d
                P.dma("sp", TAB.rr("p v h k -> p (v h k)"), tabs[g], "tab")
                wkv = load_w([(768 + g * 256, 256, 0), (1536 + g * 256, 256, 256)])
                wq = load_w([(g * 256, 256, 0)])
                for cb in range(2):
                    for t0 in range(0, TOK, 512):
                        pp = next_ps(0, 3)
                        for k in range(8):
                            P.mm(pp, wq[:, k, cb * 128:(cb + 1) * 128], hT[:, k, t0:t0 + 512], start=(k == 0), stop=(k == 7))
                        P.cp("act", qT[:, cb, t0:t0 + 512], pp)
                        pp = next_ps(0, 3)
                        for k in range(8):
                            P.mm(pp, wkv[:, k, cb * 128:(cb + 1) * 128], hT[:, k, t0:t0 + 512], start=(k == 0), stop=(k == 7))
                        P.cp("dve", kT[:, cb, t0:t0 + 512], pp)
                for r in range(d):
                    for blk in range(nblk):
                        pp = next_ps(0, 3)
                        b0 = r + d * 128 * blk
                        for k in range(8):
                            P.mm(pp[:, 0:256], hT[:, k, sstep(b0, 128, d)], wkv[:, k, 256:512], start=(k == 0), stop=(k == 7))
                        P.cp("act", Vo[:, r * nblk + blk, :], pp[:, 0:256])
                check(31)
                tiles = [(r, blk) for r in range(d) for blk in range(nblk)]
                items = [(ti, hh) for ti in range(len(tiles)) for hh in range(4)]

                def tile_ctx(ti):
                    r, blk = tiles[ti]
                    A_ = ATT[ti % 2]
                    b0 = r + d * 128 * blk
                    return r, blk, A_, PS[3 + ti % 2], sstep(b0, 128, d)

                def stA(i):
                    ti, hh = items[i]
                    r, blk, A_, Ops, qs = tile_ctx(ti)
                    NM, L_ = A_[:, 0:4], A_[:, 4:8]
                    cb, hr = hh // 2, slice((hh % 2) * 64, (hh % 2) * 64 + 64)
                    Sp = PS[5 + i % 2]
                    if blk == 0:
                        Kp = kTh[g][hr, cb, sstep(r, 128, d)]
                        var = 0
                    else:
                        Kp = kT[hr, cb, sstep(r + d * 128 * (blk - 1), 128, d)]
                        var = 1
                    Kc = kT[hr, cb, qs]
                    P.mm(Sp[:, 0:128], qT[hr, cb, qs], Kp)
                    P.mm(Sp[:, 128:256], qT[hr, cb, qs], Kc)
                    s2 = S2[i % 2]
                    P.stt("dve", s2, Sp[:, 0:256], 0.125, TAB[:, var, hh, :], ALU.mult, ALU.add)
                    P.red(NM[:, hh:hh + 1], s2, ALU.max, negate=True)
                    P.act(Pb[i % 2], s2, AF.Exp, bias=NM[:, hh:hh + 1], accum=L_[:, hh:hh + 1])

                def stC(i):
                    pb = Pb[i % 2]
                    ptp = PT[i % 4]
                    P.tr(ptp[:, 0:128], pb[:, 0:128], IDENT_B)
                    P.tr(ptp[:, 128:256], pb[:, 128:256], IDENT_B)
                    P.cp("act", PTs[i % 2], ptp)

                def stE(i):
                    ti, hh = items[i]
                    r, blk, A_, Ops, qs = tile_ctx(ti)
                    if blk == 0:
                        Vp = Vh[g][:, r, hh * 64:(hh + 1) * 64]
                    else:
                        Vp = Vo[:, r * nblk + blk - 1, hh * 64:(hh + 1) * 64]
                    Vc = Vo[:, r * nblk + blk, hh * 64:(hh + 1) * 64]
                    pts = PTs[i % 2]
                    P.mm(Ops[:, hh * 64:(hh + 1) * 64], pts[:, 0:128], Vp, start=True, stop=False)
                    P.mm(Ops[:, hh * 64:(hh + 1) * 64], pts[:, 128:256], Vc, start=False, stop=True)

                def epi1(ti):
                    r, blk, A_, Ops, qs = tile_ctx(ti)
                    NM, L_, RL, LSE = A_[:, 0:4], A_[:, 4:8], A_[:, 8:12], A_[:, 12:16]
                    P.recip(RL, L_)
                    on = On[ti % 2]
                    P.tt("dve", on[:, 0:256].rr("p (h e) -> p h e", h=4), Ops[:, 0:256].rr("p (h e) -> p h e", h=4),
                         RL.un(2).bt([128, 4, 64]), ALU.mult)
                    P.act(LSE, L_, AF.Ln)

                def epi2(ti):
                    r, blk, A_, Ops, qs = tile_ctx(ti)
                    NM, LSE = A_[:, 0:4], A_[:, 12:16]
                    on = On[ti % 2]
                    P.tt("dve", on[:, 256:260], LSE, NM, ALU.subtract)
                    px = PS[ti % 3]
                    P.tr(px[:, 0:128], on[:, 0:128], IDENT_F)
                    P.tr(px[:, 128:256], on[:, 128:256], IDENT_F)
                    P.tr(px[:, 256:384], on[:, 256:384], IDENT_F)

                def epi3(ti):
                    r, blk, A_, Ops, qs = tile_ctx(ti)
                    px = PS[ti % 3]
                    P.cp("act", OT[g][:, :, qs], px[:, 0:256].rr("p (c t) -> p c t", c=2))
                    P.cp("act", LT[g][:, qs], px[:, 256:384])

                n_it = len(items)
                for sstp in range(n_it + 5):
                    if sstp < n_it:
                        stA(sstp)
                    if 0 <= sstp - 1 < n_it:
                        stC(sstp - 1)
                    if 0 <= sstp - 2 < n_it:
                        stE(sstp - 2)
                    for lag, fn in ((2, epi1), (3, epi2), (4, epi3)):
                        j = sstp - lag
                        if 0 <= j < n_it and j % 4 == 3:
                            fn(j // 4)
            P.barrier()
            check(4)
            attnT = V(R2[:, 0:2048].bitcast(BF16).rearrange("p (c t) -> p c t", c=2), P.buf("attnT"))
            Mx = V(R2[:, 2048:4096], P.buf("Mx"))
            Wsum = V(R2[:, 4096:6144], P.buf("Wsum"))
            WBoff = [6144, 7168, 15360]
            WB = [V(R2[:, WBoff[g]:WBoff[g] + 1024].bitcast(BF16), P.buf(f"WB{g}")) for g in range(3)]
            ACC = region(R2, 14336, 512, "ACC")
            TMP = region(R2, 14848, 512, "TMP")
            P.tt("dve", Mx, LT[0], LT[1], ALU.max)
            P.tt("dve", Mx, Mx, LT[2], ALU.max)
            for g in range(3):
                P.tt("dve", LT[g], LT[g], Mx, ALU.subtract)
                P.act(LT[g], LT[g], AF.Exp)
            P.tt("dve", Wsum, LT[0], LT[1], ALU.add)
            P.tt("dve", Wsum, Wsum, LT[2], ALU.add)
            P.recip(Wsum, Wsum)
            for g in range(3):
                P.tt("dve", WB[g], LT[g], Wsum, ALU.mult)
            mi = 0
            for cc in range(2):
                for t0 in range(0, TOK, 512):
                    pg = [PS[(mi * 3 + g) % 6] for g in range(3)]
                    mi += 1
                    for g in range(3):
                        P.mm(pg[g], EBC[:, cc * 128:(cc + 1) * 128], WB[g][:, t0:t0 + 512])
                    P.tt("dve", ACC, pg[0], OT[0][:, cc, t0:t0 + 512], ALU.mult)
                    P.tt("dve", TMP, pg[1], OT[1][:, cc, t0:t0 + 512], ALU.mult)
                    P.tt("dve", ACC, ACC, TMP, ALU.add)
                    P.tt("dve", TMP, pg[2], OT[2][:, cc, t0:t0 + 512], ALU.mult)
                    P.tt("dve", attnT[:, cc, t0:t0 + 512], ACC, TMP, ALU.add)
            P.barrier()
            attn2 = V(R3[:, 0:2048].bitcast(BF16).rearrange("p (c t) -> p c t", c=2), P.buf("attn2"))
            P.cp("dve", attn2, attnT)
            P.barrier()

            check(5)
            sets = lru_views()
            lru_chunk(3, sets, own=True)
            P.barrier()
            check(6)

            WPA = V(R2[:, 0:1024].bitcast(BF16).rearrange("p (k c) -> p k c", k=2), P.buf("WPA"))
            WPL = V(R2[:, 1024:5120].bitcast(BF16).rearrange("p (k c) -> p k c", k=8), P.buf("WPL"))
            mergedT = V(R2[:, 5120:13312].bitcast(BF16).rearrange("p (k t) -> p k t", k=8), P.buf("mergedT"))
            SA = [region(R2, 13312 + i * 512, 512, f"SA{i}") for i in range(2)]
            SBg = [region(R2, 14336 + i * 512, 512, f"SB{i}") for i in range(2)]
            T1 = [region(R2, 15360 + i * 512, 512, f"T1{i}") for i in range(2)]
            P.dma("pool", WPA, dram(wpa_d, "wpa_d").rr("(k p) c -> p k c", p=128), "wpa")
            P.dma("pool", WPL, dram(wpl_d, "wpl_d").rr("(k p) c -> p k c", p=128), "wpl")
            it = 0
            for m in range(8):
                w = load_w([(C3 + m * 128, 128, 0), (C3 + 1024 + m * 128, 128, 128)])
                for t0 in range(0, TOK, 512):
                    it += 1
                    ga, gb_, pa, pl = PS[3 * (it % 2)], PS[1 + 3 * (it % 2)], PS[2 + 3 * (it % 2)], PS[6]
                    for k in range(8):
                        P.mm(ga, w[:, k, 0:128], hT[:, k, t0:t0 + 512], start=(k == 0), stop=(k == 7))
                    P.act(SA[it % 2], ga, AF.Sigmoid)
                    for k in range(8):
                        P.mm(gb_, w[:, k, 128:256], hT[:, k, t0:t0 + 512], start=(k == 0), stop=(k == 7))
                    P.act(SBg[it % 2], gb_, AF.Sigmoid)
                    for k in range(2):
                        P.mm(pa, WPA[:, k, m * 128:(m + 1) * 128], attn2[:, k, t0:t0 + 512], start=(k == 0), stop=(k == 1))
                    for k in range(8):
                        P.mm(pl, WPL[:, k, m * 128:(m + 1) * 128], lruT[:, k, t0:t0 + 512], start=(k == 0), stop=(k == 7))
                    P.tt("dve", SA[it % 2], SA[it % 2], pa, ALU.mult)
                    P.tt("dve", SBg[it % 2], SBg[it % 2], pl, ALU.mult)
                    P.tt("dve", mergedT[:, m, t0:t0 + 512], SA[it % 2], SBg[it % 2], ALU.add)
            P.barrier()

            check(7)
            WOUT = V(R3[:, 0:4096].bitcast(BF16).rearrange("p (k c) -> p k c", k=8), P.buf("WOUT"))
            P.dma("pool", WOUT, dram(wout_d, "wout_d").rr("(k p) c -> p k c", p=128), "wout")
            x1t = [V(R1[:, t * 1024:(t + 1) * 1024], P.buf(f"x1_{t}")) for t in range(16)]
            for t in range(16):
                sl = t % 2
                P.dma("sp", xs[sl], xin[3, t * 128:(t + 1) * 128, :], f"xs{sl}")
                for half in range(2):
                    pp = next_ps(0, 7)
                    for k in range(8):
                        P.mm(pp, mergedT[:, k, t * 128:(t + 1) * 128], WOUT[:, k, half * 512:(half + 1) * 512],
                             start=(k == 0), stop=(k == 7))
                    P.tt("dve", x1t[t][:, half * 512:(half + 1) * 512], pp, xs[sl][:, half * 512:(half + 1) * 512], ALU.add)
            P.barrier()

            check(8)
            load_gbc(n2_d, "n2d")
            H2 = [region(R2, i * 1024, 1024, f"H2_{i}") for i in range(2)]
            H2B = [region(R2, 2048 + i * 512, 512, f"H2B_{i}", BF16) for i in range(2)]
            H2T = [region(R2, 3072 + i * 1024, 1024, f"H2T_{i}") for i in range(2)]
            WRv = WR.rr("p (k e) -> p k e", k=8)
            G12v = G12.rr("p (t k) -> p t k", k=2)
            RSC = [(region(R2, 5120 + i * 128, 32, f"OH2_{i}"), region(R2, 5120 + i * 128 + 32, 16, f"AB_{i}", BF16),
                    region(R2, 5120 + i * 128 + 48, 32, f"POS_{i}"), region(R2, 5120 + i * 128 + 80, 32, f"JK_{i}")) for i in range(2)]
            for t in range(16):
                sl = t % 2
                h2, h2b, h2t = H2[sl], H2B[sl], H2T[sl]
                rms_tile(x1t[t], h2, gbc, h2)
                P.cp("act", h2b, h2)
                pa, pb_ = PS[0 + 2 * sl], PS[1 + 2 * sl]
                for k in range(8):
                    dst = (pa if k < 4 else pb_)[:, (k % 4) * 128:(k % 4 + 1) * 128]
                    P.tr(dst, h2[:, k * 128:(k + 1) * 128], IDENT_F)
                P.cp("act", h2t[:, 0:512], pa)
                P.cp("dve", h2t[:, 512:1024], pb_)
                pl = PS[4 + sl]
                for k in range(8):
                    P.mm(pl[:, 0:36], h2t[:, k * 128:(k + 1) * 128], WRv[:, k, :], start=(k == 0), stop=(k == 7))
                R_ = RT[sl]
                LG = R_[:, 0:36]
                GM = R_[:, 36:37]
                EG = R_[:, 37:41]
                GS = R_[:, 41:42]
                PSEL = R_[:, 42:43]
                GMASK = R_[:, 43:47]
                LS = R_[:, 47:55]
                V1 = R_[:, 55:56]
                M1 = R_[:, 56:64]
                LS2 = R_[:, 64:72]
                V2 = R_[:, 72:73]
                M2 = R_[:, 73:81]
                E2 = R_[:, 81:82]
                DEN = R_[:, 82:83]
                DST = R_[:, 83:85]
                NGM = R_[:, 85:86]
                OH = R_[:, 86:118]
                P.cp("dve", LG, pl[:, 0:36])
                P.red(GM, LG[:, 0:4], ALU.max)
                P.ts("dve", NGM, GM, -1.0, None, ALU.mult)
                P.act(EG, LG[:, 0:4], AF.Exp, bias=NGM, accum=GS)
                P.recip(PSEL, GS)
                P.ts("dve", GMASK, LG[:, 0:4], GM, None, ALU.is_equal)
                P.ts("dve", LS, LG[:, 4:12], GMASK[:, 0:1], None, ALU.mult)
                for gq in range(1, 4):
                    P.stt("dve", LS, LG[:, 4 + 8 * gq:12 + 8 * gq], GMASK[:, gq:gq + 1], LS, ALU.mult, ALU.add)
                P.red(V1, LS, ALU.max)
                P.ts("dve", M1, LS, V1, None, ALU.is_equal)
                P.stt("dve", LS2, M1, -1e30, LS, ALU.mult, ALU.add)
                P.red(V2, LS2, ALU.max)
                P.ts("dve", M2, LS2, V2, None, ALU.is_equal)
                P.tt("dve", E2, V2, V1, ALU.subtract)
                P.act(E2, E2, AF.Exp)
                P.ts("dve", DEN, E2, 1.0, None, ALU.add)
                P.recip(DEN, DEN)
                P.tt("dve", G12v[:, t, 0:1], DEN, PSEL, ALU.mult)
                P.tt("dve", G12v[:, t, 1:2], G12v[:, t, 0:1], E2, ALU.mult)
                OH2, AB, POS, JK = RSC[sl]
                P.tt("dve", OH.rr("p (g e) -> p g e", g=4), GMASK.un(2).bt([128, 4, 8]), M1.un(1).bt([128, 4, 8]), ALU.mult)
                P.tt("dve", OH2.rr("p (g e) -> p g e", g=4), GMASK.un(2).bt([128, 4, 8]), M2.un(1).bt([128, 4, 8]), ALU.mult)
                P.tt("dve", AB, OH, OH2, ALU.add)
                pc = PS[6]
                P.mm(pc[:, 0:32], UMAT, AB)
                P.mm(pc[:, 32:64], ONES_B, AB)
                P.tt("dve", POS, pc[:, 0:32], CNT, ALU.add)
                P.tt("dve", CNT, CNT, pc[:, 32:64], ALU.add)
                P.tt("dve", POS, POS, EOFF, ALU.add)
                P.tt("dve", JK, OH, POS, ALU.mult)
                P.red(DST[:, 0:1], JK, ALU.add)
                P.tt("dve", JK, OH2, POS, ALU.mult)
                P.red(DST[:, 1:2], JK, ALU.add)
                P.cp("dve", IDX[:, 2 * t:2 * t + 2], DST)
                for kk in range(2):
                    o_ap, i_ap, ix = xbuf.ap, h2b.ap, IDX.ap[:, 2 * t + kk:2 * t + kk + 1]
                    P.dma("pool", xbuf, h2b, f"scat{sl}", extra=[IDX],
                          fn=lambda e, o_ap=o_ap, i_ap=i_ap, ix=ix: e.indirect_dma_start(
                              out=o_ap, out_offset=bass.IndirectOffsetOnAxis(ap=ix, axis=0), in_=i_ap, in_offset=None))
            P.barrier()

            check(9)
            WG = [V(R2[:, i * 4096:(i + 1) * 4096].bitcast(BF16).rearrange("p (k c) -> p k c", k=8), P.buf(f"WG{i}")) for i in range(3)]
            WD = [V(R2[:, 12288 + i * 2048:12288 + (i + 1) * 2048].bitcast(BF16).rearrange("p (k c) -> p k c", k=4), P.buf(f"WD{i}")) for i in range(2)]
            WD.append(V(WS[0][:, :, :].rearrange("p k c -> p (k c)").rearrange("p (k c) -> p k c", k=4), P.buf("WD2")))
            ws1flat = WS[1][:, :, :].rearrange("p k c -> p (k c)")
            XSl = [V(ws1flat[:, i * 2048:(i + 1) * 2048].rearrange("p (s c) -> p s c", s=2), P.buf(f"XSl{i}")) for i in range(2)]
            XSl.append(V(XS[2][:, :].bitcast(BF16).rearrange("p (s c) -> p s c", s=2), P.buf("XSl2")))
            XT = [V(XS[i][:, :].bitcast(BF16).rearrange("p (k s) -> p k s", k=8), P.buf(f"XT{i}")) for i in range(2)]
            SG = [V(R3[:, i * 1024:(i + 1) * 1024].rearrange("p (m s) -> p m s", m=4), P.buf(f"SG{i}")) for i in range(2)]
            ACTT = [V(R3[:, 2048 + i * 512:2048 + (i + 1) * 512].bitcast(BF16).rearrange("p (m s) -> p m s", m=4), P.buf(f"ACTT{i}")) for i in range(2)]
            YO = [V(R3[:, 3072 + i * 1024:3072 + (i + 1) * 1024], P.buf(f"YO{i}")) for i in range(2)]
            yi = [0]

            def Wload(e_):
                sl = e_ % 3
                P.dma("pool", WG[sl], wgu[e_].rr("(k p) c -> p k c", p=128), f"wg{sl}")
                P.dma("pool", WD[sl], wdn[e_].rr("(k p) c -> p k c", p=128), f"wd{sl}")

            def Xload(e_):
                sl = e_ % 3
                P.dma("sp", XSl[sl], xbuf[e_ * CAP:(e_ + 1) * CAP, :].rr("(s p) c -> p s c", p=128), f"xsl{sl}")

            def XtrGroup(e_, gi):
                sl = e_ % 2
                x3 = e_ % 3
                s_, kh = gi // 2, gi % 2
                for k4 in range(4):
                    k = kh * 4 + k4
                    P.tr(PTall[:, k4 * 128:(k4 + 1) * 128], XSl[x3][:, s_, k * 128:(k + 1) * 128], IDENT_B)
                src = PTall[:, 0:512].rr("p (k t) -> p k t", k=4)
                if gi % 2:
                    P.cp("act", XT[sl][:, kh * 4:(kh + 1) * 4, s_ * 128:(s_ + 1) * 128], src)
                else:
                    P.cp("dve", XT[sl][:, kh * 4:(kh + 1) * 4, s_ * 128:(s_ + 1) * 128], src)

            def Xtr(e_):
                for gi in range(4):
                    XtrGroup(e_, gi)

            def GateUp(e_, nxt):
                sl = e_ % 2
                w3 = e_ % 3
                for m in range(8):
                    pp = next_ps(0, 4)
                    for k in range(8):
                        P.mm(pp[:, 0:256], WG[w3][:, k, m * 128:(m + 1) * 128], XT[sl][:, k, :], start=(k == 0), stop=(k == 7))
                    if m < 4:
                        P.act(SG[sl][:, m, :], pp[:, 0:256], AF.Silu)
                    else:
                        P.tt("dve", ACTT[sl][:, m - 4, :], SG[sl][:, m - 4, :], pp[:, 0:256], ALU.mult)

            def Down(e_):
                sl = e_ % 2
                for s_ in range(2):
                    yo = YO[yi[0] % 2]
                    yi[0] += 1
                    for half in range(2):
                        pp = PS[4 + (yi[0] + half) % 3]
                        for k in range(4):
                            P.mm(pp, ACTT[sl][:, k, s_ * 128:(s_ + 1) * 128], WD[e_ % 3][:, k, half * 512:(half + 1) * 512],
                                 start=(k == 0), stop=(k == 3))
                        if half:
                            P.cp("act", yo[:, 512:1024], pp)
                        else:
                            P.cp("dve", yo[:, 0:512], pp)
                    P.dma("sp", ybuf[e_ * CAP + s_ * 128:e_ * CAP + (s_ + 1) * 128, :], yo, f"ystore{(yi[0] - 1) % 2}")

            Wload(0)
            Wload(1)
            Xload(0)
            Xload(1)
            Xtr(0)
            for e_ in range(NEXP):
                if e_ + 2 < NEXP:
                    Wload(e_ + 2)
                    Xload(e_ + 2)
                GateUp(e_, None)
                if e_ + 1 < NEXP:
                    Xtr(e_ + 1)
                Down(e_)
            P.barrier()

            check(10)
            load_gbc(nf_d, "nfd")
            Y1 = [region(R2, i * 1024, 1024, f"Y1_{i}") for i in range(2)]
            Y2 = [region(R2, 2048 + i * 1024, 1024, f"Y2_{i}") for i in range(2)]
            ACCg = [region(R2, 4096 + i * 1024, 1024, f"ACCg_{i}") for i in range(2)]
            OUTt = [region(R2, 6144 + i * 1024, 1024, f"OUT_{i}") for i in range(2)]
            for i in range(2):
                P.memset("dve", Y1[i], 0.0)
                P.memset("dve", Y2[i], 0.0)
            for t in range(16):
                sl = t % 2
                for kk, Y in enumerate((Y1[sl], Y2[sl])):
                    o_ap, i_ap, ix = Y.ap, ybuf.ap, IDX.ap[:, 2 * t + kk:2 * t + kk + 1]
                    P.dma("pool", Y, ybuf, f"gath{kk}{sl}", extra=[IDX],
                          fn=lambda e, o_ap=o_ap, i_ap=i_ap, ix=ix: e.indirect_dma_start(
                              out=o_ap, out_offset=None, in_=i_ap, in_offset=bass.IndirectOffsetOnAxis(ap=ix, axis=0)))
                P.stt("dve", ACCg[sl], Y1[sl], G12v[:, t, 0:1], x1t[t], ALU.mult, ALU.add)
                P.stt("dve", ACCg[sl], Y2[sl], G12v[:, t, 1:2], ACCg[sl], ALU.mult, ALU.add)
                rms_tile(ACCg[sl], OUTt[sl], gbc, OUTt[sl])
                P.dma("sp", outv[t * 128:(t + 1) * 128, :], OUTt[sl], f"ostore{sl}")
            P.barrier()

        except _Stop:
            P.barrier()
        if dump:
            P.dma("sp", dram(d_r1, "d_r1"), V(R1[:, :], P.buf("r1all")), "dump1")
            P.dma("sp", dram(d_r2, "d_r2"), V(R2[:, :], P.buf("r2all")), "dump2")
            P.dma("sp", dram(d_r3, "d_r3"), V(R3[:, :], P.buf("r3all")), "dump3")
            P.dma("sp", dram(d_sm, "d_sm"), V(SMt[:, :], P.buf("small")), "dump4")
            P.dma("sp", dram(d_idx, "d_idx"), V(IDXt[:, :], P.buf("idxall")), "dump5")
            P.barrier()
        with nc.Block() as block:
            @block.tensor
            def _(e):
                P.replay("pe", e)

            @block.vector
            def _(e):
                P.replay("dve", e)

            @block.scalar
            def _(e):
                P.replay("act", e)

            @block.gpsimd
            def _(e):
                P.replay("pool", e)

            @block.sync
            def _(e):
                P.replay("sp", e)
    return nc


def _t5_bucket(dist):
    max_exact = 16
    d_f = np.maximum(dist, max_exact).astype(np.float32)
    large = max_exact + (np.log(d_f / np.float32(max_exact)) / np.float32(math.log(2048 / max_exact))
                         * np.float32(32 - max_exact)).astype(np.int32)
    large = np.minimum(large, 31)
    return np.where(dist < max_exact, dist, large)


def _bucket_tables():
    qi = np.arange(128)[:, None]
    ki = np.arange(256)[None, :]
    dist = 128 + qi - ki
    band = (dist >= 0) & (dist <= 128)
    out = []
    for d in DIL:
        b = _t5_bucket(np.maximum(dist, 0) * d)
        out.append(np.where(band, b, 32))
    return out


_NC_CACHE = {}


def prep_inputs(x, rel_bias, norm1, w_in, conv_w, conv_b, w_rg, b_rg, w_ig, b_ig, lru_lambda,
                w_proj_attn, w_proj_lru, w_out, norm2, w_router_group, w_router_expert,
                w_gate_up, w_down, norm_f):
    f32 = np.float32
    x = np.asarray(x, f32)
    rel_bias = np.asarray(rel_bias, f32)
    def pc(v):
        return np.asarray(v, f32).reshape(8, 128).T
    cw = np.asarray(conv_w, f32)[0]
    cvec = np.concatenate([pc(cw[0]), pc(cw[1]), pc(cw[2]), pc(cw[3]), pc(conv_b[0]), pc(b_rg[0]), pc(b_ig[0]),
                           pc(lru_lambda[0])], axis=1)
    cvec = np.ascontiguousarray(cvec, f32)
    wbd = np.zeros((128, 2, 8, 128), f32)
    for gi, wsrc in enumerate((np.asarray(w_rg, f32)[0], np.asarray(w_ig, f32)[0])):
        for c in range(8):
            wbd[0:64, gi, c, 0:64] = wsrc[2 * c]
            wbd[64:128, gi, c, 64:128] = wsrc[2 * c + 1]
    wbd = wbd.reshape(128, 2048)
    wr = np.concatenate([np.asarray(w_router_group, f32)[0], np.asarray(w_router_expert, f32)[0]], axis=1)
    wr = np.ascontiguousarray(wr.reshape(8, 128, 36).transpose(1, 0, 2).reshape(128, 288))
    kc_bf = np.zeros((128, 640), f32)
    kc_bf[:, 0:128] = np.eye(128, dtype=f32)
    kc_bf[:, 128:256] = np.triu(np.ones((128, 128), f32), 1)
    kc_bf[:, 256:384] = 1.0
    for hh in range(4):
        kc_bf[hh, 384 + hh * 64:384 + (hh + 1) * 64] = 1.0
    kc_f = np.zeros((128, 160), f32)
    kc_f[:, 0:128] = np.eye(128, dtype=f32)
    kc_f[:, 128:160] = (np.arange(32, dtype=f32) * CAP)[None, :]
    bt = _bucket_tables()
    rb_ext = np.concatenate([rel_bias, np.full((1, 12), NEG, f32)], axis=0)
    shared = {
        "cvec": cvec, "norm1": np.asarray(norm1, f32).reshape(1, D), "norm2": np.asarray(norm2, f32).reshape(1, D),
        "normf": np.asarray(norm_f, f32).reshape(1, D), "w_in": np.asarray(w_in, f32)[0], "wbd": wbd,
        "w_pa": np.asarray(w_proj_attn, f32)[0], "w_pl": np.asarray(w_proj_lru, f32)[0],
        "w_out": np.asarray(w_out, f32)[0], "wr": wr, "w_gu": np.asarray(w_gate_up, f32)[0],
        "w_dn": np.asarray(w_down, f32)[0], "kc_bf": kc_bf, "kc_f": kc_f,
    }
    in_maps = []
    for c in range(8):
        b, j = c // 4, c % 4
        xin = np.zeros((NCH, TOK, D), f32)
        valid = np.zeros((128, 4), f32)
        for q in range(NCH):
            src = j - 3 + q
            if src >= 0:
                xin[q] = x[b, src * TOK:(src + 1) * TOK]
                valid[:, q] = 1.0
        tabs = np.zeros((3, 128, 2, 4, 256), f32)
        for g in range(3):
            for hh in range(4):
                full = rb_ext[bt[g], 4 * g + hh]
                tabs[g, :, 1, hh, :] = full
                first = full.copy()
                if j == 0:
                    first[:, 0:128] = NEG
                tabs[g, :, 0, hh, :] = first
        m = dict(shared)
        m["xin"] = xin
        m["valid"] = valid
        m["tabs"] = tabs.reshape(3, 128, 2048)
        in_maps.append(m)
    return in_maps


def kernel(**inputs):
    f32 = np.float32
    in_maps = prep_inputs(**inputs)
    if "nc" not in _NC_CACHE:
        _NC_CACHE["nc"] = build_program()
    nc = _NC_CACHE["nc"]
    res = run_bass_kernel_spmd(nc, in_maps, core_ids=list(range(8)))
    out = np.zeros((2, 8192, D), f32)
    for c in range(8):
        b, j = c // 4, c % 4
        out[b, j * TOK:(j + 1) * TOK] = np.asarray(res.results[c]["out"], f32)
    return out
```

```python
import math
from contextlib import ExitStack

import numpy as np
import concourse.bass as bass
import concourse.mybir as mybir
from concourse.bass_utils import run_bass_kernel_spmd

F32 = mybir.dt.float32
BF16 = mybir.dt.bfloat16
U32 = mybir.dt.uint32
AF = mybir.ActivationFunctionType
ALU = mybir.AluOpType
AX = mybir.AxisListType

D = 1024
TOK = 2048
NCH = 4
C1 = 2304
C2 = 3328
C3 = 4352
DIL = (1, 4, 16)
NEG = -30000.0
EPS = 1e-6
CAP = 256
NEXP = 32


class Buf:
    __slots__ = ("name", "w", "r")

    def __init__(self, name):
        self.name = name
        self.w = None
        self.r = []


class V:
    __slots__ = ("ap", "buf")

    def __init__(self, ap, buf):
        self.ap = ap
        self.buf = buf

    def __getitem__(self, i):
        return V(self.ap[i], self.buf)

    def rr(self, pat, **kw):
        return V(self.ap.rearrange(pat, **kw), self.buf)

    def bc(self, dt):
        return V(self.ap.bitcast(dt), self.buf)

    def bt(self, shape):
        return V(self.ap.broadcast_to(shape), self.buf)

    def un(self, ax):
        return V(self.ap.unsqueeze(ax), self.buf)

    def tag(self, buf):
        return V(self.ap, buf)


def sstep(start, n, step):
    return slice(start, start + (n - 1) * step + 1, step)


def _ap(x):
    return x.ap if isinstance(x, V) else x


class Prog:
    ENGS = ("pe", "dve", "act", "pool", "sp")

    def __init__(self, nc, stack):
        self.nc = nc
        self.stack = stack
        self.sem = {e: stack.enter_context(nc.semaphore("sem_" + e)) for e in self.ENGS}
        self.cnt = {e: 0 for e in self.ENGS}
        self.stream = {e: [] for e in self.ENGS}
        self.waited = {e: {} for e in self.ENGS}
        self.dsems = {}
        self.semobj = {("e", e): self.sem[e] for e in self.ENGS}
        self.bufs = []
        self.same_engine_sync = True

    def buf(self, name):
        b = Buf(name)
        self.bufs.append(b)
        return b

    def dsem(self, name):
        if name not in self.dsems:
            h = self.stack.enter_context(self.nc.semaphore("d_" + name))
            self.dsems[name] = [h, 0]
            self.semobj[("d", name)] = h
        return self.dsems[name]

    def _collect(self, eng, ins, outs):
        ev = []
        for v in ins:
            if isinstance(v, V) and v.buf is not None and v.buf.w is not None:
                ev.append(v.buf.w)
        for v in outs:
            b = v.buf
            if b is None:
                continue
            if b.w is not None:
                ev.append(b.w)
            ev.extend(b.r)
        waits = []
        wd = self.waited[eng]
        mx = {}
        for key, val in ev:
            if key == ("e", eng) and (eng == "pe" or not self.same_engine_sync):
                continue
            if mx.get(key, 0) < val:
                mx[key] = val
        for key, val in mx.items():
            if wd.get(key, 0) >= val:
                continue
            wd[key] = val
            waits.append((key, val))
        return waits

    def _update(self, token, ins, outs):
        for v in outs:
            if v.buf is not None:
                v.buf.w = token
                v.buf.r = []
        for v in ins:
            if isinstance(v, V) and v.buf is not None:
                v.buf.r.append(token)
                if len(v.buf.r) > 64:
                    v.buf.r = v.buf.r[-64:] if False else v.buf.r

    def op(self, eng, fn, outs, ins):
        waits = self._collect(eng, ins, outs)
        self.cnt[eng] += 1
        token = (("e", eng), self.cnt[eng])
        self.stream[eng].append((waits, fn, self.sem[eng], 1))
        self._update(token, ins, outs)

    def dma(self, queue, out, in_, dname, fn=None, extra=()):
        waits = self._collect(queue, [in_] + list(extra), [out])
        d = self.dsem(dname)
        d[1] += 16
        token = (("d", dname), d[1])
        if fn is None:
            o, i = out.ap, in_.ap
            fn = lambda e: e.dma_start(out=o, in_=i)
        self.stream[queue].append((waits, fn, d[0], 16))
        self._update(token, [in_] + list(extra), [out])

    def barrier(self):
        targets = [(("e", e), self.cnt[e]) for e in self.ENGS if self.cnt[e] > 0]
        targets += [(("d", n), d[1]) for n, d in self.dsems.items() if d[1] > 0]
        for eng in self.ENGS:
            waits = []
            wd = self.waited[eng]
            for key, val in targets:
                if key == ("e", eng):
                    continue
                if wd.get(key, 0) >= val:
                    continue
                wd[key] = val
                waits.append((key, val))
            if waits:
                self.stream[eng].append((waits, None, None, 0))
        for b in self.bufs:
            b.w = None
            b.r = []

    def replay(self, eng, e):
        for waits, fn, sem, inc in self.stream[eng]:
            for key, val in waits:
                e.wait_ge(self.semobj[key], val)
            if fn is not None:
                ins = fn(e)
                ins.then_inc(sem, inc)

    def mm(self, out, lhsT, rhs, start=True, stop=True):
        o, l, r = out.ap, lhsT.ap, rhs.ap
        self.op("pe", lambda e: e.matmul(o, l, r, start=start, stop=stop), [out], [lhsT, rhs])

    def tr(self, out, in_, ident):
        o, i, d = out.ap, in_.ap, ident.ap
        self.op("pe", lambda e: e.transpose(o, i, d), [out], [in_, ident])

    def act(self, out, in_, func, bias=None, scale=None, accum=None):
        kw = {}
        ins = [in_]
        outs = [out]
        if bias is not None:
            kw["bias"] = _ap(bias)
            ins.append(bias)
        if scale is not None:
            kw["scale"] = _ap(scale)
            ins.append(scale)
        if accum is not None:
            kw["accum_out"] = accum.ap
            outs.append(accum)
        o, i = out.ap, in_.ap
        self.op("act", lambda e: e.activation(o, i, func, **kw), outs, ins)

    def ts(self, eng, out, in0, s1, s2, op0, op1=None):
        o, i = out.ap, in0.ap
        a1, a2 = _ap(s1), _ap(s2)
        if op1 is None:
            fn = lambda e: e.tensor_scalar(o, i, a1, None, op0)
        else:
            fn = lambda e: e.tensor_scalar(o, i, a1, a2, op0, op1)
        self.op(eng, fn, [out], [in0, s1, s2])

    def stt(self, eng, out, in0, scalar, in1, op0, op1):
        o, i0, i1, s = out.ap, in0.ap, in1.ap, _ap(scalar)
        self.op(eng, lambda e: e.scalar_tensor_tensor(o, i0, s, i1, op0, op1), [out], [in0, scalar, in1])

    def tt(self, eng, out, in0, in1, op):
        o, i0, i1 = out.ap, in0.ap, in1.ap
        self.op(eng, lambda e: e.tensor_tensor(o, i0, i1, op), [out], [in0, in1])

    def cp(self, eng, out, in_):
        o, i = out.ap, in_.ap
        if eng == "act":
            self.op(eng, lambda e: e.copy(o, i), [out], [in_])
        else:
            self.op(eng, lambda e: e.tensor_copy(o, i), [out], [in_])

    def red(self, out, in_, op, negate=False):
        o, i = out.ap, in_.ap
        self.op("dve", lambda e: e.tensor_reduce(o, i, AX.X, op, negate=negate), [out], [in_])

    def recip(self, out, in_):
        o, i = out.ap, in_.ap
        self.op("dve", lambda e: e.reciprocal(o, i), [out], [in_])

    def scan(self, out, d0, d1, init, op0, op1):
        o, a, b, c = out.ap, d0.ap, d1.ap, _ap(init)
        self.op("dve", lambda e: e.tensor_tensor_scan(o, a, b, c, op0, op1), [out], [d0, d1, init])

    def memset(self, eng, out, val):
        o = out.ap
        self.op(eng, lambda e: e.memset(o, val), [out], [])


class _Stop(Exception):
    pass


MARKS = []


def build_program(stage=99, dump=False):
    nc = bass.Bass("TRN2", target_bir_lowering=False)

    def din(name, shape, dt=F32):
        return nc.dram_tensor(name, shape, dt, kind="ExternalInput").ap()

    xin_d = din("xin", [NCH, TOK, D])
    tabs_d = din("tabs", [3, 128, 2048])
    valid_d = din("valid", [128, 4])
    cvec_d = din("cvec", [128, 64])
    n1_d = din("norm1", [1, D])
    n2_d = din("norm2", [1, D])
    nf_d = din("normf", [1, D])
    w_in_d = din("w_in", [D, 6400])
    wbd_d = din("wbd", [128, 2 * 8 * 128])
    wpa_d = din("w_pa", [256, D])
    wpl_d = din("w_pl", [D, D])
    wout_d = din("w_out", [D, D])
    wr_d = din("wr", [128, 8 * 36])
    wgu_d = din("w_gu", [NEXP, D, D])
    wdn_d = din("w_dn", [NEXP, 512, D])
    kcb_d = din("kc_bf", [128, 640])
    kcf_d = din("kc_f", [128, 160])
    out_d = nc.dram_tensor("out", [TOK, D], F32, kind="ExternalOutput").ap()
    xbuf_d = nc.dram_tensor("xbuf", [NEXP * CAP, D], BF16, kind="Internal").ap()
    ybuf_d = nc.dram_tensor("ybuf", [NEXP * CAP, D], F32, kind="Internal").ap()
    if dump:
        d_r1 = nc.dram_tensor("d_r1", [128, 16384], F32, kind="ExternalOutput").ap()
        d_r2 = nc.dram_tensor("d_r2", [128, 16384], F32, kind="ExternalOutput").ap()
        d_r3 = nc.dram_tensor("d_r3", [128, 6144], F32, kind="ExternalOutput").ap()
        d_sm = nc.dram_tensor("d_sm", [128, 512], F32, kind="ExternalOutput").ap()
        d_idx = nc.dram_tensor("d_idx", [128, 32], U32, kind="ExternalOutput").ap()

    with ExitStack() as stack:
        P = Prog(nc, stack)

        def sb(name, shape, dt):
            t = stack.enter_context(nc.sbuf_tensor(name, shape, dt))
            return t

        def ps(name, shape, dt):
            return stack.enter_context(nc.psum_tensor(name, shape, dt))

        def dram(ap, name):
            return V(ap, P.buf(name))

        xin = dram(xin_d, "xin")
        tabs = dram(tabs_d, "tabs")
        w_in = dram(w_in_d, "w_in")
        wgu = dram(wgu_d, "wgu")
        wdn = dram(wdn_d, "wdn")
        outv = dram(out_d, "out")
        xbuf = dram(xbuf_d, "xbuf")
        ybuf = dram(ybuf_d, "ybuf")

        R1 = sb("R1", [128, 16384], F32)
        R2 = sb("R2", [128, 16384], F32)
        R3 = sb("R3", [128, 6144], F32)
        WS = [sb(f"ws{i}", [128, 8, 512], BF16) for i in range(2)]
        XS = [sb(f"xs{i}", [128, 1024], F32) for i in range(4)]
        XNB = [sb(f"xnb{i}", [128, 1024], BF16) for i in range(4)]
        GBC = sb("gbct", [128, 1024], F32)
        CVt = sb("cv", [128, 64], F32)
        VALt = sb("val", [128, 4], F32)
        KBt = sb("kb", [128, 640], BF16)
        KFt = sb("kf", [128, 160], F32)
        WBDt = sb("wbdt", [128, 2 * 8 * 128], BF16)
        WRt = sb("wrt", [128, 8 * 36], F32)
        SMt = sb("sm", [128, 512], F32)
        IDXt = sb("idx", [128, 32], U32)

        PSt = [ps(f"ps{i}", [128, 512], F32) for i in range(7)]
        PTt = ps("pst", [128, 1024], BF16)
        PS = [V(PSt[i][:, :], P.buf(f"ps{i}")) for i in range(7)]
        PT_bufs = [P.buf(f"pst{i}") for i in range(4)]
        PT = [V(PTt[:, i * 256:(i + 1) * 256], PT_bufs[i]) for i in range(4)]
        PTall = V(PTt[:, :], PT_bufs[0])

        def region(t, c0, ncol, name, dt=F32, shape=None):
            ap = t[:, c0:c0 + ncol]
            if dt != F32:
                ap = ap.bitcast(dt)
            v = V(ap, P.buf(name))
            return v

        CV = V(CVt[:, :], P.buf("cv"))
        VAL = V(VALt[:, :], P.buf("val"))
        KB = V(KBt[:, :], P.buf("kb"))
        KF = V(KFt[:, :], P.buf("kf"))
        WBD = V(WBDt[:, :], P.buf("wbd"))
        WR = V(WRt[:, :], P.buf("wr"))
        gbc = V(GBC[:, :], P.buf("gbc"))
        ws = [V(WS[i][:, :, :], P.buf(f"ws{i}")) for i in range(2)]
        xs = [V(XS[i][:, :], P.buf(f"xs{i}")) for i in range(4)]
        xnb = [V(XNB[i][:, :], P.buf(f"xnb{i}")) for i in range(4)]
        IDX = V(IDXt[:, :], P.buf("idx"))

        IDENT_B = KB[:, 0:128]
        UMAT = KB[:, 128:256]
        ONES_B = KB[:, 256:384]
        EBC = KB[:, 384:640]
        IDENT_F = KF[:, 0:128]
        EOFF = KF[:, 128:160]

        sm_off = [0]

        def small(n, name):
            v = V(SMt[:, sm_off[0]:sm_off[0] + n], P.buf(name))
            sm_off[0] += n
            return v

        ST = small(8, "st")
        XTAIL = small(32, "xtail")
        NSP = small(16, "nsp")
        HB = small(16, "hb")
        HVAL = small(4, "hval")
        G12 = small(32, "g12")
        CNT = small(32, "cnt")
        stat = [small(8, f"stat{i}") for i in range(4)]
        ATT = [small(16, f"att{i}") for i in range(2)]
        RT = [small(128, f"rt{i}") for i in range(2)]

        P.dma("sp", CV, dram(cvec_d, "cvec_d"), "c_cv")
        P.dma("sp", VAL, dram(valid_d, "valid_d"), "c_val")
        P.dma("sp", KF, dram(kcf_d, "kcf_d"), "c_kf")
        P.dma("sp", WR, dram(wr_d, "wr_d"), "c_wr")
        P.dma("pool", KB, dram(kcb_d, "kcb_d"), "c_kb")
        P.dma("pool", WBD, dram(wbd_d, "wbd_d"), "c_wbd")
        P.memset("dve", ST, 0.0)
        P.memset("dve", XTAIL, 0.0)
        P.memset("dve", CNT, 0.0)
        P.act(NSP[:, 0:8], CV[:, 56:64], AF.Exp, scale=-1.0)
        P.act(NSP[:, 0:8], NSP[:, 0:8], AF.Ln, bias=1.0)
        P.ts("dve", NSP[:, 8:16], NSP[:, 0:8], -8.0, None, ALU.mult)
        P.ts("dve", NSP[:, 0:8], NSP[:, 0:8], -4.0, None, ALU.mult)
        P.ts("dve", HB[:, 0:8], CV[:, 40:48], 0.5, None, ALU.mult)
        P.ts("dve", HB[:, 8:16], CV[:, 48:56], 0.5, None, ALU.mult)
        P.ts("dve", HVAL, VAL, 0.5, None, ALU.mult)

        CW = CV[:, 0:32].rr("p (j c) -> p j c", j=4)
        CB = CV[:, 32:40]
        BRG = CV[:, 40:48]
        BIG_ = CV[:, 48:56]
        WBDv = WBD.rr("p (g c o) -> p g c o", g=2, c=8)

        hT = V(R1[:, 0:8192].bitcast(BF16).rearrange("p (k t) -> p k t", k=8), P.buf("hT"))
        lruT = V(R1[:, 8192:16384].bitcast(BF16).rearrange("p (k t) -> p k t", k=8), P.buf("lruT"))
        x1buf = P.buf("x1")
        x1 = V(R1[:, :].rearrange("p (t c) -> p t c", t=16), x1buf)

        r3b = R3[:, :].bitcast(BF16)
        kTh = []
        Vh = []
        off = 0
        for g in range(3):
            ext = 128 * DIL[g]
            kTh.append(V(r3b[:, off:off + 2 * ext].rearrange("p (c t) -> p c t", c=2), P.buf(f"kTh{g}")))
            off += 2 * ext
        for g in range(3):
            d = DIL[g]
            Vh.append(V(r3b[:, off:off + d * 256].rearrange("p (r c) -> p r c", r=d), P.buf(f"Vh{g}")))
            off += d * 256

        sctr = [0]

        def next_stat():
            sctr[0] += 1
            return stat[sctr[0] % 4]

        def load_gbc(src_d, nm):
            P.dma("sp", gbc, V(src_d.broadcast_to([128, D]), P.buf(nm)), "gbc")

        def rms_tile(src, dst, gb, slot_scr):
            s = next_stat()
            P.act(slot_scr, src, AF.Square, accum=s[:, 0:1])
            P.act(s[:, 1:2], s[:, 0:1], AF.Sqrt, bias=EPS, scale=1.0 / D)
            P.recip(s[:, 2:3], s[:, 1:2])
            P.stt("dve", dst, src, s[:, 2:3], gb, ALU.mult, ALU.mult)

        wsl = [0]

        def load_w(col_list, width=None):
            s = wsl[0] % 2
            wsl[0] += 1
            for (c0, n, d0) in col_list:
                src = w_in[:, c0:c0 + n].rr("(k p) c -> p k c", p=128)
                P.dma("pool", ws[s][:, :, d0:d0 + n], src, f"ws{s}")
            return ws[s]

        psr = [0]

        def next_ps(lo=0, hi=7):
            psr[0] += 1
            return PS[lo + psr[0] % (hi - lo)]

        def lru_views():
            sets = []
            o = 0
            for s in range(2):
                d_ = {}
                d_["XR"] = region(R2, o, 1032, f"XR{s}"); o += 1032
                d_["XC"] = region(R2, o, 1024, f"XC{s}"); o += 1024
                d_["RA"] = region(R2, o, 1024, f"RA{s}"); o += 1024
                d_["II"] = region(R2, o, 1024, f"II{s}"); o += 1024
                d_["TT"] = region(R2, o, 1024, f"TT{s}"); o += 1024
                d_["GL"] = region(R2, o, 1024, f"GL{s}"); o += 1024
                d_["XCB"] = region(R2, o, 512, f"XCB{s}", BF16); o += 512
                sets.append(d_)
            return sets

        def norm_chunk(q):
            def front(tt):
                sl = tt % 4
                P.dma("sp", xs[sl], xin[q, tt * 128:(tt + 1) * 128, :], f"xs{sl}")
                rms_tile(xs[sl], xnb[sl], gbc, xnb[sl])

            def back(tt):
                sl = tt % 4
                for k in range(8):
                    P.tr(PTall[:, k * 128:(k + 1) * 128], xnb[sl][:, k * 128:(k + 1) * 128], IDENT_B)
                if tt % 2:
                    P.cp("act", hT[:, :, tt * 128:(tt + 1) * 128], PTall.rr("p (k t) -> p k t", k=8))
                else:
                    P.cp("dve", hT[:, :, tt * 128:(tt + 1) * 128], PTall.rr("p (k t) -> p k t", k=8))

            front(0)
            front(1)
            for tt in range(16):
                if tt + 2 < 16:
                    front(tt + 2)
                back(tt)

        def lru_chunk(q, sets, own):
            state = {"wcur": None, "wgl": None}

            def S1(u):
                c, hf = u // 2, u % 2
                cc = c % 4
                XR = sets[u % 2]["XR"]
                t0 = hf * 1024
                if c % 4 == 0 and hf == 0:
                    state["wcur"] = load_w([(C1 + (c // 4) * 512, 512, 0)])
                wcur = state["wcur"]
                for j in range(2):
                    pp = next_ps(0, 3)
                    for k in range(8):
                        P.mm(pp, wcur[:, k, cc * 128:(cc + 1) * 128], hT[:, k, t0 + j * 512:t0 + (j + 1) * 512],
                             start=(k == 0), stop=(k == 7))
                    P.cp("act", XR[:, 3 + j * 512:3 + (j + 1) * 512], pp)

            def S2(u):
                c = u // 2
                S = sets[u % 2]
                XR, XC = S["XR"], S["XC"]
                P.cp("dve", XR[:, 0:3], XTAIL[:, c * 4:c * 4 + 3])
                P.ts("dve", XC, XR[:, 3:1027], CW[:, 3, c:c + 1], CB[:, c:c + 1], ALU.mult, ALU.add)
                for j in range(3):
                    P.stt("dve", XC, XR[:, j:j + 1024], CW[:, j, c:c + 1], XC, ALU.mult, ALU.add)
                P.cp("dve", XTAIL[:, c * 4:c * 4 + 3], XR[:, 1024:1027])

            def S3a(u):
                S = sets[u % 2]
                P.cp("act", S["XCB"], S["XC"])

            def S3b(u):
                c = u // 2
                XCB = sets[u % 2]["XCB"]
                for j in range(2):
                    P.mm(PS[3 + j], WBDv[:, 0, c, :], XCB[:, j * 512:(j + 1) * 512])
                    P.mm(PS[5 + j], WBDv[:, 1, c, :], XCB[:, j * 512:(j + 1) * 512])

            def S4(u):
                c = u // 2
                S = sets[u % 2]
                RA, II, TT = S["RA"], S["II"], S["TT"]
                for j in range(2):
                    P.act(RA[:, j * 512:(j + 1) * 512], PS[3 + j], AF.Tanh, bias=HB[:, c:c + 1], scale=0.5)
                    P.act(II[:, j * 512:(j + 1) * 512], PS[5 + j], AF.Tanh, bias=HB[:, 8 + c:9 + c], scale=0.5)
                P.act(TT, RA, AF.Exp, scale=NSP[:, 8 + c:9 + c], bias=NSP[:, 8 + c:9 + c])
                P.act(RA, RA, AF.Exp, scale=NSP[:, c:c + 1], bias=NSP[:, c:c + 1])
                P.act(TT, TT, AF.Sqrt, bias=1.0, scale=-1.0)

            def S5(u):
                c, hf = u // 2, u % 2
                cc = c % 4
                S = sets[u % 2]
                XC, RA, II, TT, GL = S["XC"], S["RA"], S["II"], S["TT"], S["GL"]
                t0 = hf * 1024
                P.stt("dve", II, II, 1.0, XC, ALU.add, ALU.mult)
                P.stt("dve", II, II, HVAL[:, q:q + 1], TT, ALU.mult, ALU.mult)
                P.scan(XC, RA, II, ST[:, c:c + 1], ALU.mult, ALU.add)
                P.cp("dve", ST[:, c:c + 1], XC[:, 1023:1024])
                if own:
                    if c % 4 == 0 and hf == 0:
                        state["wgl"] = load_w([(C2 + (c // 4) * 512, 512, 0)])
                    wgl = state["wgl"]
                    for j in range(2):
                        pp = next_ps(0, 3)
                        for k in range(8):
                            P.mm(pp, wgl[:, k, cc * 128:(cc + 1) * 128], hT[:, k, t0 + j * 512:t0 + (j + 1) * 512],
                                 start=(k == 0), stop=(k == 7))
                        P.act(GL[:, j * 512:(j + 1) * 512], pp, AF.Gelu)
                    P.tt("dve", lruT[:, c, t0:t0 + 1024], XC, GL, ALU.mult)

            S1(0)
            S2(0)
            S3a(0)
            for u in range(16):
                if u + 1 < 16:
                    S1(u + 1)
                S3b(u)
                if u + 1 < 16:
                    S2(u + 1)
                S4(u)
                if u + 1 < 16:
                    S3a(u + 1)
                S5(u)

        def halo_kv():
            for g in range(3):
                d = DIL[g]
                ext = 128 * d
                h0 = TOK - ext
                w = load_w([(768 + g * 256, 256, 0), (1536 + g * 256, 256, 256)])
                for cb in range(2):
                    for t0 in range(0, ext, 512):
                        n = min(512, ext - t0)
                        pp = next_ps(0, 3)
                        for k in range(8):
                            P.mm(pp[:, 0:n], w[:, k, cb * 128:(cb + 1) * 128], hT[:, k, h0 + t0:h0 + t0 + n],
                                 start=(k == 0), stop=(k == 7))
                        P.cp("act", kTh[g][:, cb, t0:t0 + n], pp[:, 0:n])
                for r in range(d):
                    pp = next_ps(0, 3)
                    for k in range(8):
                        P.mm(pp[:, 0:256], hT[:, k, sstep(h0 + r, 128, d)], w[:, k, 256:512],
                             start=(k == 0), stop=(k == 7))
                    P.cp("dve", Vh[g][:, r, :], pp[:, 0:256])

        def check(n):
            MARKS.append((n, dict(P.cnt)))
            if stage == n:
                raise _Stop()

        try:
            load_gbc(n1_d, "n1d")
            sets = lru_views()
            for q in range(3):
                norm_chunk(q)
                if q == 0:
                    check(1)
                lru_chunk(q, sets, own=False)
                if q == 0:
                    check(2)
                if q == 2:
                    halo_kv()
            norm_chunk(3)
            P.barrier()
            check(3)

            qT = V(R2[:, 0:2048].bitcast(BF16).rearrange("p (c t) -> p c t", c=2), P.buf("qT"))
            kT = V(R2[:, 2048:4096].bitcast(BF16).rearrange("p (c t) -> p c t", c=2), P.buf("kT"))
            Vo = V(R2[:, 4096:6144].bitcast(BF16).rearrange("p (r c) -> p r c", r=16), P.buf("Vo"))
            TAB = V(R2[:, 6144:8192].rearrange("p (v h k) -> p v h k", v=2, h=4), P.buf("TAB"))
            LT = [V(R2[:, 8192 + g * 2048:8192 + (g + 1) * 2048], P.buf(f"LT{g}")) for g in range(3)]
            o2 = 14336
            S2 = [region(R2, o2 + i * 256, 256, f"S2_{i}") for i in range(2)]
            o2 += 512
            Pb = [region(R2, o2 + i * 128, 128, f"P_{i}", BF16) for i in range(2)]
            o2 += 256
            PTs = [region(R2, o2 + i * 128, 128, f"PTs_{i}", BF16) for i in range(2)]
            o2 += 256
            On = [region(R2, o2 + i * 384, 384, f"On_{i}") for i in range(2)]
            o2 += 768
            for i in range(2):
                P.memset("dve", On[i], 0.0)
            OT = [V(lruT.ap[:, 2 * g:2 * g + 2, :], lruT.buf) for g in range(3)]

            for g in range(3):
                d = DIL[g]
                nblk = 16 // d
                P.dma("sp", TAB.rr("p v h k -> p (v h k)"), tabs[g], "tab")
                wkv = load_w([(768 + g * 256, 256, 0), (1536 + g * 256, 256, 256)])
                wq = load_w([(g * 256, 256, 0)])
                for cb in range(2):
                    for t0 in range(0, TOK, 512):
                        pp = next_ps(0, 3)
                        for k in range(8):
                            P.mm(pp, wq[:, k, cb * 128:(cb + 1) * 128], hT[:, k, t0:t0 + 512], start=(k == 0), stop=(k == 7))
                        P.cp("act", qT[:, cb, t0:t0 + 512], pp)
                        pp = next_ps(0, 3)
                        for k in range(8):
                            P.mm(pp, wkv[:, k, cb * 128:(cb + 1) * 128], hT[:, k, t0:t0 + 512], start=(k == 0), stop=(k == 7))
                        P.cp("dve", kT[:, cb, t0:t0 + 512], pp)
                for r in range(d):
                    for blk in range(nblk):
                        pp = next_ps(0, 3)
                        b0 = r + d * 128 * blk
                        for k in range(8):
                            P.mm(pp[:, 0:256], hT[:, k, sstep(b0, 128, d)], wkv[:, k, 256:512], start=(k == 0), stop=(k == 7))
                        P.cp("act", Vo[:, r * nblk + blk, :], pp[:, 0:256])
                check(31)
                tiles = [(r, blk) for r in range(d) for blk in range(nblk)]
                items = [(ti, hh) for ti in range(len(tiles)) for hh in range(4)]

                def tile_ctx(ti):
                    r, blk = tiles[ti]
                    A_ = ATT[ti % 2]
                    b0 = r + d * 128 * blk
                    return r, blk, A_, PS[3 + ti % 2], sstep(b0, 128, d)

                def stA(i):
                    ti, hh = items[i]
                    r, blk, A_, Ops, qs = tile_ctx(ti)
                    NM, L_ = A_[:, 0:4], A_[:, 4:8]
                    cb, hr = hh // 2, slice((hh % 2) * 64, (hh % 2) * 64 + 64)
                    Sp = PS[5 + i % 2]
                    if blk == 0:
                        Kp = kTh[g][hr, cb, sstep(r, 128, d)]
                        var = 0
                    else:
                        Kp = kT[hr, cb, sstep(r + d * 128 * (blk - 1), 128, d)]
                        var = 1
                    Kc = kT[hr, cb, qs]
                    P.mm(Sp[:, 0:128], qT[hr, cb, qs], Kp)
                    P.mm(Sp[:, 128:256], qT[hr, cb, qs], Kc)
                    s2 = S2[i % 2]
                    P.stt("dve", s2, Sp[:, 0:256], 0.125, TAB[:, var, hh, :], ALU.mult, ALU.add)
                    P.red(NM[:, hh:hh + 1], s2, ALU.max, negate=True)
                    P.act(Pb[i % 2], s2, AF.Exp, bias=NM[:, hh:hh + 1], accum=L_[:, hh:hh + 1])

                def stC(i):
                    pb = Pb[i % 2]
                    ptp = PT[i % 4]
                    P.tr(ptp[:, 0:128], pb[:, 0:128], IDENT_B)
                    P.tr(ptp[:, 128:256], pb[:, 128:256], IDENT_B)
                    P.cp("act", PTs[i % 2], ptp)

                def stE(i):
                    ti, hh = items[i]
                    r, blk, A_, Ops, qs = tile_ctx(ti)
                    if blk == 0:
                        Vp = Vh[g][:, r, hh * 64:(hh + 1) * 64]
                    else:
                        Vp = Vo[:, r * nblk + blk - 1, hh * 64:(hh + 1) * 64]
                    Vc = Vo[:, r * nblk + blk, hh * 64:(hh + 1) * 64]
                    pts = PTs[i % 2]
                    P.mm(Ops[:, hh * 64:(hh + 1) * 64], pts[:, 0:128], Vp, start=True, stop=False)
                    P.mm(Ops[:, hh * 64:(hh + 1) * 64], pts[:, 128:256], Vc, start=False, stop=True)

                def epi1(ti):
                    r, blk, A_, Ops, qs = tile_ctx(ti)
                    NM, L_, RL, LSE = A_[:, 0:4], A_[:, 4:8], A_[:, 8:12], A_[:, 12:16]
                    P.recip(RL, L_)
                    on = On[ti % 2]
                    P.tt("dve", on[:, 0:256].rr("p (h e) -> p h e", h=4), Ops[:, 0:256].rr("p (h e) -> p h e", h=4),
                         RL.un(2).bt([128, 4, 64]), ALU.mult)
                    P.act(LSE, L_, AF.Ln)

                def epi2(ti):
                    r, blk, A_, Ops, qs = tile_ctx(ti)
                    NM, LSE = A_[:, 0:4], A_[:, 12:16]
                    on = On[ti % 2]
                    P.tt("dve", on[:, 256:260], LSE, NM, ALU.subtract)
                    px = PS[ti % 3]
                    P.tr(px[:, 0:128], on[:, 0:128], IDENT_F)
                    P.tr(px[:, 128:256], on[:, 128:256], IDENT_F)
                    P.tr(px[:, 256:384], on[:, 256:384], IDENT_F)

                def epi3(ti):
                    r, blk, A_, Ops, qs = tile_ctx(ti)
                    px = PS[ti % 3]
                    P.cp("act", OT[g][:, :, qs], px[:, 0:256].rr("p (c t) -> p c t", c=2))
                    P.cp("act", LT[g][:, qs], px[:, 256:384])

                n_it = len(items)
                for sstp in range(n_it + 5):
                    if sstp < n_it:
                        stA(sstp)
                    if 0 <= sstp - 1 < n_it:
                        stC(sstp - 1)
                    if 0 <= sstp - 2 < n_it:
                        stE(sstp - 2)
                    for lag, fn in ((2, epi1), (3, epi2), (4, epi3)):
                        j = sstp - lag
                        if 0 <= j < n_it and j % 4 == 3:
                            fn(j // 4)
            P.barrier()
            check(4)
            attnT = V(R2[:, 0:2048].bitcast(BF16).rearrange("p (c t) -> p c t", c=2), P.buf("attnT"))
            Mx = V(R2[:, 2048:4096], P.buf("Mx"))
            Wsum = V(R2[:, 4096:6144], P.buf("Wsum"))
            WBoff = [6144, 7168, 15360]
            WB = [V(R2[:, WBoff[g]:WBoff[g] + 1024].bitcast(BF16), P.buf(f"WB{g}")) for g in range(3)]
            ACC = region(R2, 14336, 512, "ACC")
            TMP = region(R2, 14848, 512, "TMP")
            P.tt("dve", Mx, LT[0], LT[1], ALU.max)
            P.tt("dve", Mx, Mx, LT[2], ALU.max)
            for g in range(3):
                P.tt("dve", LT[g], LT[g], Mx, ALU.subtract)
                P.act(LT[g], LT[g], AF.Exp)
            P.tt("dve", Wsum, LT[0], LT[1], ALU.add)
            P.tt("dve", Wsum, Wsum, LT[2], ALU.add)
            P.recip(Wsum, Wsum)
            for g in range(3):
                P.tt("dve", WB[g], LT[g], Wsum, ALU.mult)
            mi = 0
            for cc in range(2):
                for t0 in range(0, TOK, 512):
                    pg = [PS[(mi * 3 + g) % 6] for g in range(3)]
                    mi += 1
                    for g in range(3):
                        P.mm(pg[g], EBC[:, cc * 128:(cc + 1) * 128], WB[g][:, t0:t0 + 512])
                    P.tt("dve", ACC, pg[0], OT[0][:, cc, t0:t0 + 512], ALU.mult)
                    P.tt("dve", TMP, pg[1], OT[1][:, cc, t0:t0 + 512], ALU.mult)
                    P.tt("dve", ACC, ACC, TMP, ALU.add)
                    P.tt("dve", TMP, pg[2], OT[2][:, cc, t0:t0 + 512], ALU.mult)
                    P.tt("dve", attnT[:, cc, t0:t0 + 512], ACC, TMP, ALU.add)
            P.barrier()
            attn2 = V(R3[:, 0:2048].bitcast(BF16).rearrange("p (c t) -> p c t", c=2), P.buf("attn2"))
            P.cp("dve", attn2, attnT)
            P.barrier()

            check(5)
            sets = lru_views()
            lru_chunk(3, sets, own=True)
            P.barrier()
            check(6)

            WPA = V(R2[:, 0:1024].bitcast(BF16).rearrange("p (k c) -> p k c", k=2), P.buf("WPA"))
            WPL = V(R2[:, 1024:5120].bitcast(BF16).rearrange("p (k c) -> p k c", k=8), P.buf("WPL"))
            mergedT = V(R2[:, 5120:13312].bitcast(BF16).rearrange("p (k t) -> p k t", k=8), P.buf("mergedT"))
            SA = [region(R2, 13312 + i * 512, 512, f"SA{i}") for i in range(2)]
            SBg = [region(R2, 14336 + i * 512, 512, f"SB{i}") for i in range(2)]
            T1 = [region(R2, 15360 + i * 512, 512, f"T1{i}") for i in range(2)]
            P.dma("pool", WPA, dram(wpa_d, "wpa_d").rr("(k p) c -> p k c", p=128), "wpa")
            P.dma("pool", WPL, dram(wpl_d, "wpl_d").rr("(k p) c -> p k c", p=128), "wpl")
            it = 0
            for m in range(8):
                w = load_w([(C3 + m * 128, 128, 0), (C3 + 1024 + m * 128, 128, 128)])
                for t0 in range(0, TOK, 512):
                    it += 1
                    ga, gb_, pa, pl = PS[3 * (it % 2)], PS[1 + 3 * (it % 2)], PS[2 + 3 * (it % 2)], PS[6]
                    for k in range(8):
                        P.mm(ga, w[:, k, 0:128], hT[:, k, t0:t0 + 512], start=(k == 0), stop=(k == 7))
                    P.act(SA[it % 2], ga, AF.Sigmoid)
                    for k in range(8):
                        P.mm(gb_, w[:, k, 128:256], hT[:, k, t0:t0 + 512], start=(k == 0), stop=(k == 7))
                    P.act(SBg[it % 2], gb_, AF.Sigmoid)
                    for k in range(2):
                        P.mm(pa, WPA[:, k, m * 128:(m + 1) * 128], attn2[:, k, t0:t0 + 512], start=(k == 0), stop=(k == 1))
                    for k in range(8):
                        P.mm(pl, WPL[:, k, m * 128:(m + 1) * 128], lruT[:, k, t0:t0 + 512], start=(k == 0), stop=(k == 7))
                    P.tt("dve", SA[it % 2], SA[it % 2], pa, ALU.mult)
                    P.tt("dve", SBg[it % 2], SBg[it % 2], pl, ALU.mult)
                    P.tt("dve", mergedT[:, m, t0:t0 + 512], SA[it % 2], SBg[it % 2], ALU.add)
            P.barrier()

            check(7)
            WOUT = V(R3[:, 0:4096].bitcast(BF16).rearrange("p (k c) -> p k c", k=8), P.buf("WOUT"))
            P.dma("pool", WOUT, dram(wout_d, "wout_d").rr("(k p) c -> p k c", p=128), "wout")
            x1t = [V(R1[:, t * 1024:(t + 1) * 1024], P.buf(f"x1_{t}")) for t in range(16)]
            for t in range(16):
                sl = t % 2
                P.dma("sp", xs[sl], xin[3, t * 128:(t + 1) * 128, :], f"xs{sl}")
                for half in range(2):
                    pp = next_ps(0, 7)
                    for k in range(8):
                        P.mm(pp, mergedT[:, k, t * 128:(t + 1) * 128], WOUT[:, k, half * 512:(half + 1) * 512],
                             start=(k == 0), stop=(k == 7))
                    P.tt("dve", x1t[t][:, half * 512:(half + 1) * 512], pp, xs[sl][:, half * 512:(half + 1) * 512], ALU.add)
            P.barrier()

            check(8)
            load_gbc(n2_d, "n2d")
            H2 = [region(R2, i * 1024, 1024, f"H2_{i}") for i in range(2)]
            H2B = [region(R2, 2048 + i * 512, 512, f"H2B_{i}", BF16) for i in range(2)]
            H2T = [region(R2, 3072 + i * 1024, 1024, f"H2T_{i}") for i in range(2)]
            WRv = WR.rr("p (k e) -> p k e", k=8)
            G12v = G12.rr("p (t k) -> p t k", k=2)
            RSC = [(region(R2, 5120 + i * 128, 32, f"OH2_{i}"), region(R2, 5120 + i * 128 + 32, 16, f"AB_{i}", BF16),
                    region(R2, 5120 + i * 128 + 48, 32, f"POS_{i}"), region(R2, 5120 + i * 128 + 80, 32, f"JK_{i}")) for i in range(2)]
            for t in range(16):
                sl = t % 2
                h2, h2b, h2t = H2[sl], H2B[sl], H2T[sl]
                rms_tile(x1t[t], h2, gbc, h2)
                P.cp("act", h2b, h2)
                pa, pb_ = PS[0 + 2 * sl], PS[1 + 2 * sl]
                for k in range(8):
                    dst = (pa if k < 4 else pb_)[:, (k % 4) * 128:(k % 4 + 1) * 128]
                    P.tr(dst, h2[:, k * 128:(k + 1) * 128], IDENT_F)
                P.cp("act", h2t[:, 0:512], pa)
                P.cp("dve", h2t[:, 512:1024], pb_)
                pl = PS[4 + sl]
                for k in range(8):
                    P.mm(pl[:, 0:36], h2t[:, k * 128:(k + 1) * 128], WRv[:, k, :], start=(k == 0), stop=(k == 7))
                R_ = RT[sl]
                LG = R_[:, 0:36]
                GM = R_[:, 36:37]
                EG = R_[:, 37:41]
                GS = R_[:, 41:42]
                PSEL = R_[:, 42:43]
                GMASK = R_[:, 43:47]
                LS = R_[:, 47:55]
                V1 = R_[:, 55:56]
                M1 = R_[:, 56:64]
                LS2 = R_[:, 64:72]
                V2 = R_[:, 72:73]
                M2 = R_[:, 73:81]
                E2 = R_[:, 81:82]
                DEN = R_[:, 82:83]
                DST = R_[:, 83:85]
                NGM = R_[:, 85:86]
                OH = R_[:, 86:118]
                P.cp("dve", LG, pl[:, 0:36])
                P.red(GM, LG[:, 0:4], ALU.max)
                P.ts("dve", NGM, GM, -1.0, None, ALU.mult)
                P.act(EG, LG[:, 0:4], AF.Exp, bias=NGM, accum=GS)
                P.recip(PSEL, GS)
                P.ts("dve", GMASK, LG[:, 0:4], GM, None, ALU.is_equal)
                P.ts("dve", LS, LG[:, 4:12], GMASK[:, 0:1], None, ALU.mult)
                for gq in range(1, 4):
                    P.stt("dve", LS, LG[:, 4 + 8 * gq:12 + 8 * gq], GMASK[:, gq:gq + 1], LS, ALU.mult, ALU.add)
                P.red(V1, LS, ALU.max)
                P.ts("dve", M1, LS, V1, None, ALU.is_equal)
                P.stt("dve", LS2, M1, -1e30, LS, ALU.mult, ALU.add)
                P.red(V2, LS2, ALU.max)
                P.ts("dve", M2, LS2, V2, None, ALU.is_equal)
                P.tt("dve", E2, V2, V1, ALU.subtract)
                P.act(E2, E2, AF.Exp)
                P.ts("dve", DEN, E2, 1.0, None, ALU.add)
                P.recip(DEN, DEN)
                P.tt("dve", G12v[:, t, 0:1], DEN, PSEL, ALU.mult)
                P.tt("dve", G12v[:, t, 1:2], G12v[:, t, 0:1], E2, ALU.mult)
                OH2, AB, POS, JK = RSC[sl]
                P.tt("dve", OH.rr("p (g e) -> p g e", g=4), GMASK.un(2).bt([128, 4, 8]), M1.un(1).bt([128, 4, 8]), ALU.mult)
                P.tt("dve", OH2.rr("p (g e) -> p g e", g=4), GMASK.un(2).bt([128, 4, 8]), M2.un(1).bt([128, 4, 8]), ALU.mult)
                P.tt("dve", AB, OH, OH2, ALU.add)
                pc = PS[6]
                P.mm(pc[:, 0:32], UMAT, AB)
                P.mm(pc[:, 32:64], ONES_B, AB)
                P.tt("dve", POS, pc[:, 0:32], CNT, ALU.add)
                P.tt("dve", CNT, CNT, pc[:, 32:64], ALU.add)
                P.tt("dve", POS, POS, EOFF, ALU.add)
                P.tt("dve", JK, OH, POS, ALU.mult)
                P.red(DST[:, 0:1], JK, ALU.add)
                P.tt("dve", JK, OH2, POS, ALU.mult)
                P.red(DST[:, 1:2], JK, ALU.add)
                P.cp("dve", IDX[:, 2 * t:2 * t + 2], DST)
                for kk in range(2):
                    o_ap, i_ap, ix = xbuf.ap, h2b.ap, IDX.ap[:, 2 * t + kk:2 * t + kk + 1]
                    P.dma("pool", xbuf, h2b, f"scat{sl}", extra=[IDX],
                          fn=lambda e, o_ap=o_ap, i_ap=i_ap, ix=ix: e.indirect_dma_start(
                              out=o_ap, out_offset=bass.IndirectOffsetOnAxis(ap=ix, axis=0), in_=i_ap, in_offset=None))
            P.barrier()

            check(9)
            WG = [V(R2[:, i * 4096:(i + 1) * 4096].bitcast(BF16).rearrange("p (k c) -> p k c", k=8), P.buf(f"WG{i}")) for i in range(3)]
            WD = [V(R2[:, 12288 + i * 2048:12288 + (i + 1) * 2048].bitcast(BF16).rearrange("p (k c) -> p k c", k=4), P.buf(f"WD{i}")) for i in range(2)]
            WD.append(V(WS[0][:, :, :].rearrange("p k c -> p (k c)").rearrange("p (k c) -> p k c", k=4), P.buf("WD2")))
            ws1flat = WS[1][:, :, :].rearrange("p k c -> p (k c)")
            XSl = [V(ws1flat[:, i * 2048:(i + 1) * 2048].rearrange("p (s c) -> p s c", s=2), P.buf(f"XSl{i}")) for i in range(2)]
            XSl.append(V(XS[2][:, :].bitcast(BF16).rearrange("p (s c) -> p s c", s=2), P.buf("XSl2")))
            XT = [V(XS[i][:, :].bitcast(BF16).rearrange("p (k s) -> p k s", k=8), P.buf(f"XT{i}")) for i in range(2)]
            SG = [V(R3[:, i * 1024:(i + 1) * 1024].rearrange("p (m s) -> p m s", m=4), P.buf(f"SG{i}")) for i in range(2)]
            ACTT = [V(R3[:, 2048 + i * 512:2048 + (i + 1) * 512].bitcast(BF16).rearrange("p (m s) -> p m s", m=4), P.buf(f"ACTT{i}")) for i in range(2)]
            YO = [V(R3[:, 3072 + i * 1024:3072 + (i + 1) * 1024], P.buf(f"YO{i}")) for i in range(2)]
            yi = [0]

            def Wload(e_):
                sl = e_ % 3
                P.dma("pool", WG[sl], wgu[e_].rr("(k p) c -> p k c", p=128), f"wg{sl}")
                P.dma("pool", WD[sl], wdn[e_].rr("(k p) c -> p k c", p=128), f"wd{sl}")

            def Xload(e_):
                sl = e_ % 3
                P.dma("sp", XSl[sl], xbuf[e_ * CAP:(e_ + 1) * CAP, :].rr("(s p) c -> p s c", p=128), f"xsl{sl}")

            def XtrGroup(e_, gi):
                sl = e_ % 2
                x3 = e_ % 3
                s_, kh = gi // 2, gi % 2
                for k4 in range(4):
                    k = kh * 4 + k4
                    P.tr(PTall[:, k4 * 128:(k4 + 1) * 128], XSl[x3][:, s_, k * 128:(k + 1) * 128], IDENT_B)
                src = PTall[:, 0:512].rr("p (k t) -> p k t", k=4)
                if gi % 2:
                    P.cp("act", XT[sl][:, kh * 4:(kh + 1) * 4, s_ * 128:(s_ + 1) * 128], src)
                else:
                    P.cp("dve", XT[sl][:, kh * 4:(kh + 1) * 4, s_ * 128:(s_ + 1) * 128], src)

            def Xtr(e_):
                for gi in range(4):
                    XtrGroup(e_, gi)

            def GateUp(e_, nxt):
                sl = e_ % 2
                w3 = e_ % 3
                for m in range(8):
                    pp = next_ps(0, 4)
                    for k in range(8):
                        P.mm(pp[:, 0:256], WG[w3][:, k, m * 128:(m + 1) * 128], XT[sl][:, k, :], start=(k == 0), stop=(k == 7))
                    if m < 4:
                        P.act(SG[sl][:, m, :], pp[:, 0:256], AF.Silu)
                    else:
                        P.tt("dve", ACTT[sl][:, m - 4, :], SG[sl][:, m - 4, :], pp[:, 0:256], ALU.mult)

            def Down(e_):
                sl = e_ % 2
                for s_ in range(2):
                    yo = YO[yi[0] % 2]
                    yi[0] += 1
                    for half in range(2):
                        pp = PS[4 + (yi[0] + half) % 3]
                        for k in range(4):
                            P.mm(pp, ACTT[sl][:, k, s_ * 128:(s_ + 1) * 128], WD[e_ % 3][:, k, half * 512:(half + 1) * 512],
                                 start=(k == 0), stop=(k == 3))
                        if half:
                            P.cp("act", yo[:, 512:1024], pp)
                        else:
                            P.cp("dve", yo[:, 0:512], pp)
                    P.dma("sp", ybuf[e_ * CAP + s_ * 128:e_ * CAP + (s_ + 1) * 128, :], yo, f"ystore{(yi[0] - 1) % 2}")

            Wload(0)
            Wload(1)
            Xload(0)
            Xload(1)
            Xtr(0)
            for e_ in range(NEXP):
                if e_ + 2 < NEXP:
                    Wload(e_ + 2)
                    Xload(e_ + 2)
                GateUp(e_, None)
                if e_ + 1 < NEXP:
                    Xtr(e_ + 1)
                Down(e_)
            P.barrier()

            check(10)
            load_gbc(nf_d, "nfd")
            Y1 = [region(R2, i * 1024, 1024, f"Y1_{i}") for i in range(2)]
            Y2 = [region(R2, 2048 + i * 1024, 1024, f"Y2_{i}") for i in range(2)]
            ACCg = [region(R2, 4096 + i * 1024, 1024, f"ACCg_{i}") for i in range(2)]
            OUTt = [region(R2, 6144 + i * 1024, 1024, f"OUT_{i}") for i in range(2)]
            for i in range(2):
                P.memset("dve", Y1[i], 0.0)
                P.memset("dve", Y2[i], 0.0)
            for t in range(16):
                sl = t % 2
                for kk, Y in enumerate((Y1[sl], Y2[sl])):
                    o_ap, i_ap, ix = Y.ap, ybuf.ap, IDX.ap[:, 2 * t + kk:2 * t + kk + 1]
                    P.dma("pool", Y, ybuf, f"gath{kk}{sl}", extra=[IDX],
                          fn=lambda e, o_ap=o_ap, i_ap=i_ap, ix=ix: e.indirect_dma_start(
                              out=o_ap, out_offset=None, in_=i_ap, in_offset=bass.IndirectOffsetOnAxis(ap=ix, axis=0)))
                P.stt("dve", ACCg[sl], Y1[sl], G12v[:, t, 0:1], x1t[t], ALU.mult, ALU.add)
                P.stt("dve", ACCg[sl], Y2[sl], G12v[:, t, 1:2], ACCg[sl], ALU.mult, ALU.add)
                rms_tile(ACCg[sl], OUTt[sl], gbc, OUTt[sl])
                P.dma("sp", outv[t * 128:(t + 1) * 128, :], OUTt[sl], f"ostore{sl}")
            P.barrier()

        except _Stop:
            P.barrier()
        if dump:
            P.dma("sp", dram(d_r1, "d_r1"), V(R1[:, :], P.buf("r1all")), "dump1")
            P.dma("sp", dram(d_r2, "d_r2"), V(R2[:, :], P.buf("r2all")), "dump2")
            P.dma("sp", dram(d_r3, "d_r3"), V(R3[:, :], P.buf("r3all")), "dump3")
            P.dma("sp", dram(d_sm, "d_sm"), V(SMt[:, :], P.buf("small")), "dump4")
            P.dma("sp", dram(d_idx, "d_idx"), V(IDXt[:, :], P.buf("idxall")), "dump5")
            P.barrier()
        with nc.Block() as block:
            @block.tensor
            def _(e):
                P.replay("pe", e)

            @block.vector
            def _(e):
                P.replay("dve", e)

            @block.scalar
            def _(e):
                P.replay("act", e)

            @block.gpsimd
            def _(e):
                P.replay("pool", e)

            @block.sync
            def _(e):
                P.replay("sp", e)
    return nc


def _t5_bucket(dist):
    max_exact = 16
    d_f = np.maximum(dist, max_exact).astype(np.float32)
    large = max_exact + (np.log(d_f / np.float32(max_exact)) / np.float32(math.log(2048 / max_exact))
                         * np.float32(32 - max_exact)).astype(np.int32)
    large = np.minimum(large, 31)
    return np.where(dist < max_exact, dist, large)


def _bucket_tables():
    qi = np.arange(128)[:, None]
    ki = np.arange(256)[None, :]
    dist = 128 + qi - ki
    band = (dist >= 0) & (dist <= 128)
    out = []
    for d in DIL:
        b = _t5_bucket(np.maximum(dist, 0) * d)
        out.append(np.where(band, b, 32))
    return out


_NC_CACHE = {}


def prep_inputs(x, rel_bias, norm1, w_in, conv_w, conv_b, w_rg, b_rg, w_ig, b_ig, lru_lambda,
                w_proj_attn, w_proj_lru, w_out, norm2, w_router_group, w_router_expert,
                w_gate_up, w_down, norm_f):
    f32 = np.float32
    x = np.asarray(x, f32)
    rel_bias = np.asarray(rel_bias, f32)
    def pc(v):
        return np.asarray(v, f32).reshape(8, 128).T
    cw = np.asarray(conv_w, f32)[0]
    cvec = np.concatenate([pc(cw[0]), pc(cw[1]), pc(cw[2]), pc(cw[3]), pc(conv_b[0]), pc(b_rg[0]), pc(b_ig[0]),
                           pc(lru_lambda[0])], axis=1)
    cvec = np.ascontiguousarray(cvec, f32)
    wbd = np.zeros((128, 2, 8, 128), f32)
    for gi, wsrc in enumerate((np.asarray(w_rg, f32)[0], np.asarray(w_ig, f32)[0])):
        for c in range(8):
            wbd[0:64, gi, c, 0:64] = wsrc[2 * c]
            wbd[64:128, gi, c, 64:128] = wsrc[2 * c + 1]
    wbd = wbd.reshape(128, 2048)
    wr = np.concatenate([np.asarray(w_router_group, f32)[0], np.asarray(w_router_expert, f32)[0]], axis=1)
    wr = np.ascontiguousarray(wr.reshape(8, 128, 36).transpose(1, 0, 2).reshape(128, 288))
    kc_bf = np.zeros((128, 640), f32)
    kc_bf[:, 0:128] = np.eye(128, dtype=f32)
    kc_bf[:, 128:256] = np.triu(np.ones((128, 128), f32), 1)
    kc_bf[:, 256:384] = 1.0
    for hh in range(4):
        kc_bf[hh, 384 + hh * 64:384 + (hh + 1) * 64] = 1.0
    kc_f = np.zeros((128, 160), f32)
    kc_f[:, 0:128] = np.eye(128, dtype=f32)
    kc_f[:, 128:160] = (np.arange(32, dtype=f32) * CAP)[None, :]
    bt = _bucket_tables()
    rb_ext = np.concatenate([rel_bias, np.full((1, 12), NEG, f32)], axis=0)
    shared = {
        "cvec": cvec, "norm1": np.asarray(norm1, f32).reshape(1, D), "norm2": np.asarray(norm2, f32).reshape(1, D),
        "normf": np.asarray(norm_f, f32).reshape(1, D), "w_in": np.asarray(w_in, f32)[0], "wbd": wbd,
        "w_pa": np.asarray(w_proj_attn, f32)[0], "w_pl": np.asarray(w_proj_lru, f32)[0],
        "w_out": np.asarray(w_out, f32)[0], "wr": wr, "w_gu": np.asarray(w_gate_up, f32)[0],
        "w_dn": np.asarray(w_down, f32)[0], "kc_bf": kc_bf, "kc_f": kc_f,
    }
    in_maps = []
    for c in range(8):
        b, j = c // 4, c % 4
        xin = np.zeros((NCH, TOK, D), f32)
        valid = np.zeros((128, 4), f32)
        for q in range(NCH):
            src = j - 3 + q
            if src >= 0:
                xin[q] = x[b, src * TOK:(src + 1) * TOK]
                valid[:, q] = 1.0
        tabs = np.zeros((3, 128, 2, 4, 256), f32)
        for g in range(3):
            for hh in range(4):
                full = rb_ext[bt[g], 4 * g + hh]
                tabs[g, :, 1, hh, :] = full
                first = full.copy()
                if j == 0:
                    first[:, 0:128] = NEG
                tabs[g, :, 0, hh, :] = first
        m = dict(shared)
        m["xin"] = xin
        m["valid"] = valid
        m["tabs"] = tabs.reshape(3, 128, 2048)
        in_maps.append(m)
    return in_maps


def kernel(**inputs):
    f32 = np.float32
    in_maps = prep_inputs(**inputs)
    if "nc" not in _NC_CACHE:
        _NC_CACHE["nc"] = build_program()
    nc = _NC_CACHE["nc"]
    res = run_bass_kernel_spmd(nc, in_maps, core_ids=list(range(8)))
    out = np.zeros((2, 8192, D), f32)
    for c in range(8):
        b, j = c // 4, c % 4
        out[b, j * TOK:(j + 1) * TOK] = np.asarray(res.results[c]["out"], f32)
    return out
```
